# Optimizing a Trainium2 kernel written in Bass

```python
import math
import jax
import jax.numpy as jnp
from jax import lax
import numpy as np

D_MODEL = 1024
BATCH = 8
SEQ = 2048
DEPTH = 4

SSD_HEADS = 16
SSD_HEAD_DIM = 64
SSD_INNER = SSD_HEADS * SSD_HEAD_DIM
SSD_GROUPS = 2
SSD_STATE = 128
SSD_CONV = 4
SSD_CHUNK = 128
S5_GROUPS = 32
S5_GROUP_CH = 16
S5_WIDTH = S5_GROUPS * S5_GROUP_CH
S5_STATE = 64
MIX_WIDTH = SSD_INNER + S5_WIDTH
CONV_CH = SSD_INNER + 2 * SSD_GROUPS * SSD_STATE
IN_COLS = SSD_INNER + CONV_CH + SSD_HEADS + S5_WIDTH
N_EXPERT_GROUPS = 4
EXPERTS_PER_GROUP = 8
N_EXPERTS = N_EXPERT_GROUPS * EXPERTS_PER_GROUP
TOP_K = 2
EXPERT_FF = 512
MOE_BLOCK = 128
EPS = 1e-6

kernel_name = 'hybrid_ssd_s5_hmoe_block'


def rmsnorm(x, w):
    xf = x.astype(jnp.float32)
    y = xf * lax.rsqrt(jnp.mean(xf * xf, axis=-1, keepdims=True) + EPS)
    return y.astype(x.dtype) * w


def modulate(h, shift, scale):
    return h * (1.0 + scale[:, None, :]) + shift[:, None, :]


def causal_depthwise_conv(u, w, b):
    k, ch = w.shape
    y = lax.conv_general_dilated(u, w[:, None, :], (1,), [(k - 1, 0)],
                                 dimension_numbers=('NWC', 'WIO', 'NWC'),
                                 feature_group_count=ch)
    return y + b


def ssd_chunked_scan(x, dt, a, bm, cm):
    b, s, h, p = x.shape
    g, n = bm.shape[2], bm.shape[3]
    r = h // g
    nc = s // SSD_CHUNK
    xd = (x * dt[..., None]).reshape(b, nc, SSD_CHUNK, g, r, p)
    ad = (dt * a).reshape(b, nc, SSD_CHUNK, g, r)
    bc = bm.reshape(b, nc, SSD_CHUNK, g, n)
    cc = cm.reshape(b, nc, SSD_CHUNK, g, n)
    a_cs = jnp.cumsum(ad, axis=2)
    seg = a_cs[:, :, :, None] - a_cs[:, :, None, :]
    causal = jnp.tril(jnp.ones((SSD_CHUNK, SSD_CHUNK), bool))[None, None, :, :, None, None]
    decay = jnp.exp(jnp.where(causal, seg, -jnp.inf))
    cb = jnp.einsum('bclgn,bcsgn->bclsg', cc, bc)
    y_diag = jnp.einsum('bclsgr,bcsgrp->bclgrp', decay * cb[..., None], xd)
    decay_states = jnp.exp(a_cs[:, :, -1:] - a_cs)
    states = jnp.einsum('bclgn,bclgrp->bcgrpn', bc, xd * decay_states[..., None])
    chunk_decay = jnp.exp(a_cs[:, :, -1])

    def step(hc, inp):
        st, dc = inp
        return hc * dc[..., None, None] + st, hc

    h0 = jnp.zeros((b, g, r, p, n), states.dtype)
    _, h_prev = lax.scan(step, h0, (jnp.moveaxis(states, 1, 0), jnp.moveaxis(chunk_decay, 1, 0)))
    h_prev = jnp.moveaxis(h_prev, 0, 1)
    y_off = jnp.einsum('bclgn,bcgrpn->bclgrp', cc, h_prev) * jnp.exp(a_cs)[..., None]
    return (y_diag + y_off).reshape(b, s, h, p)


def ssd_mixer(z, xbc, dt_raw, conv_w, conv_b, dt_bias, a_log, d_skip, norm_w):
    bsz, seqlen, _ = z.shape
    f32 = jnp.float32
    xbc = jax.nn.silu(causal_depthwise_conv(xbc, conv_w, conv_b))
    nb = SSD_GROUPS * SSD_STATE
    xs = xbc[..., :SSD_INNER].reshape(bsz, seqlen, SSD_HEADS, SSD_HEAD_DIM).astype(f32)
    bm = xbc[..., SSD_INNER:SSD_INNER + nb].reshape(bsz, seqlen, SSD_GROUPS, SSD_STATE).astype(f32)
    cm = xbc[..., SSD_INNER + nb:].reshape(bsz, seqlen, SSD_GROUPS, SSD_STATE).astype(f32)
    dt = jax.nn.softplus((dt_raw + dt_bias).astype(f32))
    a = -jnp.exp(a_log.astype(f32))
    y = ssd_chunked_scan(xs, dt, a, bm, cm) + d_skip.astype(f32)[:, None] * xs
    y = y.reshape(bsz, seqlen, SSD_INNER) * jax.nn.silu(z.astype(f32))
    yg = y.reshape(bsz, seqlen, SSD_GROUPS, SSD_INNER // SSD_GROUPS)
    yg = yg * lax.rsqrt(jnp.mean(yg * yg, axis=-1, keepdims=True) + EPS)
    return yg.reshape(bsz, seqlen, SSD_INNER).astype(z.dtype) * norm_w


def complex_linear_combine(e1, e2):
    a1r, a1i, b1r, b1i = e1
    a2r, a2i, b2r, b2i = e2
    return (a2r * a1r - a2i * a1i, a2r * a1i + a2i * a1r,
            a2r * b1r - a2i * b1i + b2r, a2r * b1i + a2i * b1r + b2i)


def s5_mixer(u, lam_re, lam_im, log_dt, b_re, b_im, c_re, c_im, d_skip, w_glu, b_glu, norm_w):
    bsz, seqlen, _ = u.shape
    f32 = jnp.float32
    uf = u.astype(f32)
    ug = uf.reshape(bsz, seqlen, S5_GROUPS, S5_GROUP_CH)
    lr, li = lam_re.astype(f32), lam_im.astype(f32)
    step = jnp.exp(log_dt.astype(f32))[:, None]
    mag = jnp.exp(lr * step)
    ab_re, ab_im = mag * jnp.cos(li * step), mag * jnp.sin(li * step)
    den = lr * lr + li * li
    nr = ab_re - 1.0
    coef_re = (nr * lr + ab_im * li) / den
    coef_im = (ab_im * lr - nr * li) / den
    br, bi = b_re.astype(f32), b_im.astype(f32)
    bb_re = coef_re[..., None] * br - coef_im[..., None] * bi
    bb_im = coef_re[..., None] * bi + coef_im[..., None] * br
    bu_re = jnp.einsum('bsgh,gph->bsgp', ug, bb_re)
    bu_im = jnp.einsum('bsgh,gph->bsgp', ug, bb_im)
    a_re = jnp.broadcast_to(ab_re, (1, seqlen, S5_GROUPS, S5_STATE))
    a_im = jnp.broadcast_to(ab_im, (1, seqlen, S5_GROUPS, S5_STATE))
    _, _, s_re, s_im = lax.associative_scan(complex_linear_combine, (a_re, a_im, bu_re, bu_im), axis=1)
    y = (jnp.einsum('ghp,bsgp->bsgh', c_re.astype(f32), s_re)
         - jnp.einsum('ghp,bsgp->bsgh', c_im.astype(f32), s_im))
    y = y.reshape(bsz, seqlen, S5_WIDTH) + d_skip.astype(f32) * uf
    y = jax.nn.gelu(y)
    y = y * jax.nn.sigmoid(y @ w_glu.astype(f32) + b_glu.astype(f32))
    return rmsnorm(y.astype(u.dtype), norm_w)


def hybrid_mixer(h, w_in, conv_w, conv_b, dt_bias, a_log, d_ssd, ssd_norm_w,
                 lam_re, lam_im, log_dt, b_re, b_im, c_re, c_im, s5_d, w_glu, b_glu, s5_norm_w, w_out):
    proj = h @ w_in
    i0 = SSD_INNER
    i1 = i0 + CONV_CH
    i2 = i1 + SSD_HEADS
    z, xbc, dt_raw, u5 = proj[..., :i0], proj[..., i0:i1], proj[..., i1:i2], proj[..., i2:]
    y_ssd = ssd_mixer(z, xbc, dt_raw, conv_w, conv_b, dt_bias, a_log, d_ssd, ssd_norm_w)
    y_s5 = s5_mixer(u5, lam_re, lam_im, log_dt, b_re, b_im, c_re, c_im, s5_d, w_glu, b_glu, s5_norm_w)
    return jnp.concatenate([y_ssd, y_s5], axis=-1) @ w_out


def hier_moe(h, w_rg, b_rg, w_re, b_re, w_eg, w_eu, w_ed):
    bsz, seqlen, d = h.shape
    t = bsz * seqlen
    hf = h.reshape(t, d)
    g_logits = (hf @ w_rg + b_rg).astype(jnp.float32)
    g_prob = jax.nn.softmax(g_logits, axis=-1)
    g_idx = jnp.argmax(g_logits, axis=-1).astype(jnp.int32)
    g_w = jnp.take_along_axis(g_prob, g_idx[:, None], axis=1)
    e_logits = (hf @ w_re + b_re).astype(jnp.float32).reshape(t, N_EXPERT_GROUPS, EXPERTS_PER_GROUP)
    e_logits = jnp.take_along_axis(e_logits, g_idx[:, None, None], axis=1)[:, 0]
    e_prob = jax.nn.softmax(e_logits, axis=-1)
    top_p, top_i = lax.top_k(e_prob, TOP_K)
    gate = g_w * top_p / jnp.sum(top_p, axis=-1, keepdims=True)
    expert_id = (g_idx[:, None] * EXPERTS_PER_GROUP + top_i).reshape(-1).astype(jnp.int32)
    n = t * TOP_K
    token_id = jnp.arange(n, dtype=jnp.int32) // TOP_K
    order = jnp.argsort(expert_id)
    sorted_e = expert_id[order]
    counts = jnp.bincount(expert_id, length=N_EXPERTS).astype(jnp.int32)
    starts = jnp.cumsum(counts) - counts
    padded = (counts + MOE_BLOCK - 1) // MOE_BLOCK * MOE_BLOCK
    padded_end = jnp.cumsum(padded)
    padded_start = padded_end - padded
    dest = padded_start[sorted_e] + jnp.arange(n, dtype=jnp.int32) - starts[sorted_e]
    n_pad = n + N_EXPERTS * MOE_BLOCK
    n_blocks = n_pad // MOE_BLOCK
    row_token = jnp.zeros((n_pad,), jnp.int32).at[dest].set(token_id[order])
    block_start = jnp.arange(n_blocks, dtype=jnp.int32) * MOE_BLOCK
    block_expert = jnp.minimum(jnp.searchsorted(padded_end, block_start, side='right'),
                               N_EXPERTS - 1).astype(jnp.int32)
    x_rows = hf[row_token].reshape(n_blocks, MOE_BLOCK, d)

    def expert_block(args):
        xb, e = args
        return (jax.nn.silu(xb @ w_eg[e]) * (xb @ w_eu[e])) @ w_ed[e]

    y_rows = lax.map(expert_block, (x_rows, block_expert)).reshape(n_pad, d)
    slot_dest = jnp.zeros((n,), jnp.int32).at[order].set(dest)
    y_slots = y_rows[slot_dest].reshape(t, TOP_K, d)
    out = jnp.einsum('tkd,tk->td', y_slots, gate.astype(h.dtype))
    return out.reshape(bsz, seqlen, d)


def setup_inputs(seed: int = 0) -> dict:
    key = jax.random.key(seed)
    ks = jax.random.split(key, 40)
    f32 = jnp.float32
    L = DEPTH

    def nrm(k, shape, std):
        return std * jax.random.normal(k, shape, f32)

    x = nrm(ks[0], (BATCH, SEQ, D_MODEL), 1.0)
    c = nrm(ks[1], (BATCH, D_MODEL), 1.0)
    w_ada = nrm(ks[2], (L, D_MODEL, 6 * D_MODEL), 0.5 * D_MODEL ** -0.5)
    b_ada = nrm(ks[3], (L, 6 * D_MODEL), 0.02)
    norm1_w = 1.0 + nrm(ks[4], (L, D_MODEL), 0.02)
    w_in = nrm(ks[5], (L, D_MODEL, IN_COLS), D_MODEL ** -0.5)
    conv_w = nrm(ks[6], (L, SSD_CONV, CONV_CH), SSD_CONV ** -0.5)
    conv_b = nrm(ks[7], (L, CONV_CH), 0.02)
    dt0 = jnp.exp(jax.random.uniform(ks[8], (L, SSD_HEADS), f32, math.log(1e-3), math.log(1e-1)))
    dt_bias = dt0 + jnp.log(-jnp.expm1(-dt0))
    a_log = jnp.log(jax.random.uniform(ks[9], (L, SSD_HEADS), f32, 1.0, 16.0))
    d_ssd = 1.0 + nrm(ks[10], (L, SSD_HEADS), 0.02)
    ssd_norm_w = 1.0 + nrm(ks[11], (L, SSD_INNER), 0.02)
    s5_lam_re = -0.5 + nrm(ks[12], (L, S5_GROUPS, S5_STATE), 0.01)
    s5_lam_im = math.pi * jnp.arange(S5_STATE, dtype=f32) + nrm(ks[13], (L, S5_GROUPS, S5_STATE), 0.01)
    s5_log_dt = jax.random.uniform(ks[14], (L, S5_GROUPS), f32, math.log(1e-3), math.log(1e-1))
    s5_b_re = nrm(ks[15], (L, S5_GROUPS, S5_STATE, S5_GROUP_CH), (2 * S5_GROUP_CH) ** -0.5)
    s5_b_im = nrm(ks[16], (L, S5_GROUPS, S5_STATE, S5_GROUP_CH), (2 * S5_GROUP_CH) ** -0.5)
    s5_c_re = nrm(ks[17], (L, S5_GROUPS, S5_GROUP_CH, S5_STATE), (2 * S5_STATE) ** -0.5)
    s5_c_im = nrm(ks[18], (L, S5_GROUPS, S5_GROUP_CH, S5_STATE), (2 * S5_STATE) ** -0.5)
    s5_d = nrm(ks[19], (L, S5_WIDTH), 1.0)
    w_glu = nrm(ks[20], (L, S5_WIDTH, S5_WIDTH), S5_WIDTH ** -0.5)
    b_glu = nrm(ks[21], (L, S5_WIDTH), 0.02)
    s5_norm_w = 1.0 + nrm(ks[22], (L, S5_WIDTH), 0.02)
    w_out = nrm(ks[23], (L, MIX_WIDTH, D_MODEL), MIX_WIDTH ** -0.5)
    norm2_w = 1.0 + nrm(ks[24], (L, D_MODEL), 0.02)
    w_rg = nrm(ks[25], (L, D_MODEL, N_EXPERT_GROUPS), D_MODEL ** -0.5)
    b_rg = nrm(ks[26], (L, N_EXPERT_GROUPS), 0.01)
    w_re = nrm(ks[27], (L, D_MODEL, N_EXPERTS), D_MODEL ** -0.5)
    b_re = nrm(ks[28], (L, N_EXPERTS), 0.01)
    w_eg = nrm(ks[29], (L, N_EXPERTS, D_MODEL, EXPERT_FF), D_MODEL ** -0.5)
    w_eu = nrm(ks[30], (L, N_EXPERTS, D_MODEL, EXPERT_FF), D_MODEL ** -0.5)
    w_ed = nrm(ks[31], (L, N_EXPERTS, EXPERT_FF, D_MODEL), EXPERT_FF ** -0.5)
    final_norm_w = 1.0 + nrm(ks[32], (D_MODEL,), 0.02)
    return {'x': x, 'c': c, 'w_ada': w_ada, 'b_ada': b_ada, 'norm1_w': norm1_w, 'w_in': w_in,
            'conv_w': conv_w, 'conv_b': conv_b, 'dt_bias': dt_bias, 'a_log': a_log, 'd_ssd': d_ssd,
            'ssd_norm_w': ssd_norm_w, 's5_lam_re': s5_lam_re, 's5_lam_im': s5_lam_im,
            's5_log_dt': s5_log_dt, 's5_b_re': s5_b_re, 's5_b_im': s5_b_im, 's5_c_re': s5_c_re,
            's5_c_im': s5_c_im, 's5_d': s5_d, 'w_glu': w_glu, 'b_glu': b_glu, 's5_norm_w': s5_norm_w,
            'w_out': w_out, 'norm2_w': norm2_w, 'w_rg': w_rg, 'b_rg': b_rg, 'w_re': w_re, 'b_re': b_re,
            'w_eg': w_eg, 'w_eu': w_eu, 'w_ed': w_ed, 'final_norm_w': final_norm_w}


def reference(x, c, w_ada, b_ada, norm1_w, w_in, conv_w, conv_b, dt_bias, a_log, d_ssd, ssd_norm_w,
              s5_lam_re, s5_lam_im, s5_log_dt, s5_b_re, s5_b_im, s5_c_re, s5_c_im, s5_d, w_glu, b_glu,
              s5_norm_w, w_out, norm2_w, w_rg, b_rg, w_re, b_re, w_eg, w_eu, w_ed, final_norm_w):
    c_act = jax.nn.silu(c)
    for l in range(DEPTH):
        ada = c_act @ w_ada[l] + b_ada[l]
        shift_m, scale_m, gate_m, shift_f, scale_f, gate_f = jnp.split(ada, 6, axis=-1)
        h = modulate(rmsnorm(x, norm1_w[l]), shift_m, scale_m)
        m = hybrid_mixer(h, w_in[l], conv_w[l], conv_b[l], dt_bias[l], a_log[l], d_ssd[l], ssd_norm_w[l],
                         s5_lam_re[l], s5_lam_im[l], s5_log_dt[l], s5_b_re[l], s5_b_im[l], s5_c_re[l],
                         s5_c_im[l], s5_d[l], w_glu[l], b_glu[l], s5_norm_w[l], w_out[l])
        x = x + gate_m[:, None, :] * m
        h = modulate(rmsnorm(x, norm2_w[l]), shift_f, scale_f)
        f = hier_moe(h, w_rg[l], b_rg[l], w_re[l], b_re[l], w_eg[l], w_eu[l], w_ed[l])
        x = x + gate_f[:, None, :] * f
    return rmsnorm(x, final_norm_w)
```

```python
import numpy as np
from contextlib import ExitStack
import concourse.bass as bass
import concourse.mybir as mybir
from concourse.bass_utils import run_bass_kernel_spmd

F32 = mybir.dt.float32
BF16 = mybir.dt.bfloat16
AF = mybir.ActivationFunctionType
ALU = mybir.AluOpType
AX = mybir.AxisListType

D = 1024
SEQ = 2048
NT = SEQ // 128
DEPTH = 4
INC = 3088
EPS = 1e-6
MAGIC = 12582912.0
TWO_PI = float(2 * np.pi)
RW1 = 16 + 16 + 16 + 512 + 512 + 36
RW2 = 32 + 2048 + 2048


class Reg:
    __slots__ = ("name", "w", "rs")

    def __init__(self, name=""):
        self.name = name
        self.w = None
        self.rs = {}


class Tok:
    __slots__ = ("sem", "val", "eng", "key")

    def __init__(self, sem, val, eng, key):
        self.sem, self.val, self.eng, self.key = sem, val, eng, key


class Stream:
    def __init__(self, name, eng):
        self.name, self.eng = name, eng
        self.sem = None
        self.key = None
        self.count = 0
        self.known = {}


class TK:
    def __init__(self, nc, es):
        self.nc, self.es = nc, es
        self.nsem = 0
        self.S = {}
        for n, e in (("pe", nc.tensor), ("act", nc.scalar), ("dve", nc.vector), ("pool", nc.gpsimd), ("sp", nc.sync)):
            s = Stream(n, e)
            self.S[n] = s
            self._rot(s)
        self.dsems = {}
        self.dpos = {}
        for q in ("sp", "pool", "act"):
            self.dsems[q] = [[self._new_sem("d" + q), 0, self._newkey()] for _ in range(6)]
            self.dpos[q] = 0
        self.ninst = 0

    def _newkey(self):
        self.nsem += 1
        return self.nsem

    def _new_sem(self, name):
        return self.es.enter_context(self.nc.semaphore(f"{name}_{self.nsem}"))

    def _rot(self, s):
        s.key = self._newkey()
        s.sem = self._new_sem("s" + s.name)
        s.count = 0

    def _wait(self, s, tok):
        if s.known.get(tok.key, 0) >= tok.val:
            return
        s.eng.wait_ge(tok.sem, tok.val)
        s.known[tok.key] = tok.val

    def _deps(self, s, R, W):
        for r in R:
            if r.w is not None:
                t = r.w
                if not (t.eng == s.name and s.name == "pe"):
                    self._wait(s, t)
        for w in W:
            if w.w is not None:
                t = w.w
                if not (t.eng == s.name and s.name == "pe"):
                    self._wait(s, t)
            for t in w.rs.values():
                if t.eng == s.name and s.name == "pe":
                    continue
                self._wait(s, t)

    def _mark(self, tok, R, W, rkey):
        for r in R:
            r.rs[rkey] = tok
        for w in W:
            w.w = tok
            w.rs = {}

    def op(self, en, fn, R, W):
        s = self.S[en]
        R = [getattr(r, "reg", r) for r in R]
        W = [getattr(w, "reg", w) for w in W]
        self._deps(s, R, W)
        ins = fn(s.eng)
        s.count += 1
        ins.then_inc(s.sem, 1)
        tok = Tok(s.sem, s.count, en, s.key)
        s.known[s.key] = s.count if en == "pe" else s.known.get(s.key, 0)
        self._mark(tok, R, W, en)
        self.ninst += 1
        if s.count >= 30000:
            self._rot(s)
        return tok

    def dma(self, q, out, in_, R, W):
        s = self.S[q]
        R = [getattr(r, "reg", r) for r in R]
        W = [getattr(w, "reg", w) for w in W]
        self._deps(s, R, W)
        lst = self.dsems[q]
        i = self.dpos[q]
        self.dpos[q] = (i + 1) % len(lst)
        ent = lst[i]
        if ent[1] > 0:
            self._wait(s, Tok(ent[0], ent[1], "dma", ent[2]))
        if ent[1] >= 30000:
            ent[0] = self._new_sem("d" + q)
            ent[1] = 0
            ent[2] = self._newkey()
        s.eng.dma_start(out=out, in_=in_).then_inc(ent[0], 16)
        ent[1] += 16
        tok = Tok(ent[0], ent[1], "dma", ent[2])
        self._mark(tok, R, W, ("d", ent[2]))
        self.ninst += 1
        return tok

    def barrier(self):
        toks = []
        for s in self.S.values():
            if s.count > 0:
                toks.append(Tok(s.sem, s.count, s.name, s.key))
        for q, lst in self.dsems.items():
            for ent in lst:
                if ent[1] > 0:
                    toks.append(Tok(ent[0], ent[1], "dma", ent[2]))
        for s in self.S.values():
            for t in toks:
                if t.eng == s.name:
                    continue
                self._wait(s, t)


class T:
    def __init__(self, h, name):
        self.h = h
        self.reg = Reg(name)

    def __getitem__(self, k):
        return self.h[k]


class B:
    def __init__(self, nlayers):
        self.nl = nlayers
        self.nc = bass.Bass("TRN2", target_bir_lowering=False)
        self.es = ExitStack()
        self.dr = {}

    def din(self, name, shape, dt=F32):
        self.dr[name] = self.nc.dram_tensor(name, list(shape), dt, kind="ExternalInput").ap()
        return self.dr[name]

    def sb(self, st, name, shape, dt=F32):
        self.uid = getattr(self, "uid", 0) + 1
        name = f"{name}_{self.uid}"
        return T(st.enter_context(self.nc.sbuf_tensor(name, list(shape), dt)), name)

    def tt(self, en, out, in0, in1, op, R, W):
        return self.tk.op(en, lambda e: e.tensor_tensor(out=out, in0=in0, in1=in1, op=op), R, W)

    def ts(self, en, out, in0, s1, s2, op0, op1, R, W):
        if op1 is None:
            return self.tk.op(en, lambda e: e.tensor_scalar(out=out, in0=in0, scalar1=s1, scalar2=None, op0=op0), R, W)
        return self.tk.op(en, lambda e: e.tensor_scalar(out=out, in0=in0, scalar1=s1, scalar2=s2, op0=op0, op1=op1), R, W)

    def stt(self, out, in0, sc, in1, op0, op1, R, W):
        return self.tk.op("dve", lambda e: e.scalar_tensor_tensor(out=out, in0=in0, scalar=sc, in1=in1, op0=op0, op1=op1), R, W)

    def cp(self, en, out, in_, R, W):
        if en == "act":
            return self.tk.op(en, lambda e: e.copy(out=out, in_=in_), R, W)
        return self.tk.op(en, lambda e: e.tensor_copy(out=out, in_=in_), R, W)

    def act(self, out, in_, func, R, W, bias=None, scale=None, accum=None):
        kw = {}
        if bias is not None:
            kw["bias"] = bias
        if scale is not None:
            kw["scale"] = scale
        if accum is not None:
            kw["accum_out"] = accum
        return self.tk.op("act", lambda e: e.activation(out=out, in_=in_, func=func, **kw), R, W)

    def mm(self, out, pairs, R, W):
        n = len(pairs)

        def fn(e):
            ins = None
            for i, (l, r) in enumerate(pairs):
                ins = e.matmul(out, lhsT=l, rhs=r, start=(i == 0), stop=(i == n - 1))
            return ins
        return self.tk.op("pe", fn, R, W)

    def mms(self, items, R, W):
        def fn(e):
            ins = None
            for (o, l, r) in items:
                ins = e.matmul(o, lhsT=l, rhs=r, start=True, stop=True)
            return ins
        return self.tk.op("pe", fn, R, W)

    def trs(self, items, ident, R, W):
        def fn(e):
            ins = None
            for (o, i) in items:
                ins = e.transpose(o, i, ident)
            return ins
        return self.tk.op("pe", fn, R, W)

    def pb(self):
        i = self.pbi
        self.pbi = (i + 1) % 7
        return i

    def rstd(self, ss, tmp, out, n, R, W):
        self.ts("dve", tmp, ss, 1.0 / n, EPS, ALU.mult, ALU.add, R, W)
        self.act(tmp, tmp, AF.Sqrt, W, W)
        self.tk.op("dve", lambda e: e.reciprocal(out=out, in_=tmp), W, W)

    def rangered(self, en, r, x, tmp, R):
        self.ts(en, tmp[0], x, 1.0 / TWO_PI, MAGIC, ALU.mult, ALU.add, R, [tmp[1]])
        self.ts(en, tmp[0], tmp[0], MAGIC, -TWO_PI, ALU.subtract, ALU.mult, [tmp[1]], [tmp[1]])
        self.tt(en, r[0], x, tmp[0], ALU.add, R + [tmp[1]], [r[1]])
        self.ts(en, r[0], r[0], 3.141592, -3.141592, ALU.min, ALU.max, [r[1]], [r[1]])

    def build(self):
        nc, es = self.nc, self.es
        din = self.din
        x_in = din("x", [SEQ, D])
        ccol = din("ccol", [128, 8])
        w_ada = din("w_ada", [DEPTH, D, 6 * D])
        badac = din("badac", [128, DEPTH, 48])
        n1c = din("n1c", [128, DEPTH, 8])
        n2c = din("n2c", [128, DEPTH, 8])
        w_in = din("w_in", [DEPTH, D, INC])
        w_out = din("w_out", [DEPTH, 1536, D])
        w_glu = din("w_glu", [DEPTH, 512, 512])
        convw = din("convw", [128, DEPTH, 12, 4])
        convb = din("convb", [128, DEPTH, 12])
        ynormc = din("ynormc", [128, DEPTH, 12])
        rows1 = din("rows1", [DEPTH, RW1])
        rows2 = din("rows2", [DEPTH, RW2])
        fnw = din("fnw", [1, D])
        lamc = din("lamc", [128, DEPTH, 3, 16])
        bblk = din("bblk", [DEPTH, 2, 128, 16, 128])
        cblk = din("cblk", [DEPTH, 2, 128, 16, 32])
        wr = din("wr", [DEPTH, D, 36])
        w_eg = din("w_eg", [DEPTH, 32, D, 512])
        w_eu = din("w_eu", [DEPTH, 32, D, 512])
        w_ed = din("w_ed", [DEPTH, 32, 512, D])
        cst = din("cst", [128, 4, 128])
        y_out = nc.dram_tensor("y", [SEQ, D], F32, kind="ExternalOutput").ap()
        xd = nc.dram_tensor("xd", [SEQ, D], F32, kind="Internal").ap()
        self.XD = [Reg(f"xd{t}") for t in range(NT)]
        self.YD = [Reg(f"yd{t}") for t in range(NT)]

        self.tk = tk = TK(nc, es)
        self.pbi = 0
        sb = self.sb
        self.ps = ps = es.enter_context(nc.psum_tensor("ps", [128, 8, 512], F32))
        self.PB = PB = [Reg(f"pb{i}") for i in range(8)]
        G = es
        cstt = sb(G, "cstt", [128, 4, 128])
        identb = sb(G, "identb", [128, 128], BF16)
        triub = sb(G, "triub", [128, 128], BF16)
        onesf = sb(G, "onesf", [128, 128])
        adac = sb(G, "adac", [128, DEPTH, 48])
        n1t = sb(G, "n1t", [128, DEPTH, 8])
        n2t = sb(G, "n2t", [128, DEPTH, 8])
        cact = sb(G, "cact", [128, 8])
        self.cact, self.w_ada, self.badac = cact, w_ada, badac
        tk.dma("sp", cstt[:], cst[:, :, :], [], [cstt])
        tk.dma("sp", n1t[:], n1c[:, :, :], [], [n1t])
        tk.dma("sp", n2t[:], n2c[:, :, :], [], [n2t])
        tk.dma("sp", cact[:], ccol[:, :], [], [cact])
        identf = cstt[:, 0, :]
        triuf = cstt[:, 1, :]
        maskb = cstt[:, 2, :]
        iotar = cstt[:, 3, :]
        self.cp("dve", identb[:], identf, [cstt], [identb])
        self.cp("dve", triub[:], triuf, [cstt], [triub])
        tk.op("dve", lambda e: e.memset(onesf[:], 1.0), [], [onesf])
        self.act(cact[:], cact[:], AF.Silu, [cact], [cact])
        self.identf, self.identb, self.triuf, self.triub = identf, identb, triuf, triub
        self.maskb, self.iotar, self.onesf, self.cstt = maskb, iotar, onesf, cstt
        self.adac, self.n1t, self.n2t = adac, n1t, n2t

        with ExitStack() as P0:
            wblk = [sb(P0, f"wblk{i}", [128, 8, 2048]) for i in range(2)]
            arow = sb(P0, "arow", [1, 6 * D])
            bad = sb(P0, "bad", [128, 48])
            tk.dma("sp", bad[:], badac[:, 0, :], [], [bad])
            n = 0
            for l in range(1):
                for j3 in range(3):
                    wb = wblk[n % 2]
                    n += 1
                    for kq in range(4):
                        src = w_ada[l, kq * 256:(kq + 1) * 256, j3 * 2048:(j3 + 1) * 2048].rearrange("(k p) n -> p k n", p=128)
                        tk.dma("sp" if kq % 2 else "act", wb[:, kq * 2:(kq + 1) * 2, :], src, [], [wb])
                    for jb in range(4):
                        bank = self.pb()
                        self.mm(ps[0:1, bank, :], [(cact[:, k:k + 1], wb[:, k, jb * 512:(jb + 1) * 512]) for k in range(8)], [wb, cact], [PB[bank]])
                        c0 = j3 * 2048 + jb * 512
                        self.cp("act", arow[0:1, c0:c0 + 512], ps[0:1, bank, :], [PB[bank]], [arow])
                bank = self.pb()
                self.mms([(ps[:, bank, j:j + 1], arow[0:1, j * 128:(j + 1) * 128], onesf[0:1, 0:1]) for j in range(48)], [arow, onesf], [PB[bank]])
                self.tt("dve", adac[:, l, :], ps[:, bank, 0:48], bad[:], ALU.add, [PB[bank], bad], [adac])
            tk.barrier()

        src_x = x_in
        for l in range(self.nl):
            self.mixer_phase(l, src_x, xd)
            src_x = xd
            self.moe_phase(l, xd, y_out if l == self.nl - 1 else xd, fnw, l == self.nl - 1)
        s = tk.S["sp"]
        for r in self.YD:
            if r.w is not None:
                tk._wait(s, r.w)
        tk.barrier()
        es.close()
        return nc

    def bcast_cols(self, col8, dst, R):
        tk, ps, PB = self.tk, self.ps, self.PB
        for h in range(2):
            bank = self.pb()
            for kk in range(4):
                k = h * 4 + kk
                self.ts("dve", self.dall[:, kk, :], self.identf, col8[:, k:k + 1], None, ALU.mult, None, R + [self.cstt], [self.dall])
                self.mm(ps[:, bank, kk * 128:(kk + 1) * 128], [(self.onesf[:], self.dall[:, kk, :])], [self.onesf, self.dall], [PB[bank]])
            self.cp("act", dst[:, h * 512:(h + 1) * 512], ps[:, bank, :], [PB[bank]], [dst])

    def mixer_phase(self, l, xsrc, xdst):
        nc, tk, ps, PB = self.nc, self.tk, self.ps, self.PB
        sb = self.sb
        dr = self.dr
        adac = self.adac
        with ExitStack() as M:
            Win = sb(M, "Win", [128, 8, INC], BF16)
            Wout = sb(M, "Wout", [128, 12, D], BF16)
            Wglu = sb(M, "Wglu", [128, 4, 512], BF16)
            Qc = sb(M, "Qc", [128, 16, 128])
            Qs = sb(M, "Qs", [128, 16, 128])
            Bb = sb(M, "Bb", [128, 16, 2, 128], BF16)
            Cb = sb(M, "Cb", [128, 16, 2, 32], BF16)
            mcol = sb(M, "mcol", [128, 16])
            r1 = sb(M, "r1", [128, RW1])
            A1c = sb(M, "A1c", [128, 8])
            abc = sb(M, "abc", [128, 16])
            cw = sb(M, "cw", [128, 12, 4])
            cbt = sb(M, "cbt", [128, 12])
            ync = sb(M, "ync", [128, 12])
            for k in range(8):
                tk.dma("pool", Win[:, k, :], dr["w_in"][l, k * 128:(k + 1) * 128, :], [], [Win])
            for k in range(12):
                tk.dma("pool", Wout[:, k, :], dr["w_out"][l, k * 128:(k + 1) * 128, :], [], [Wout])
            tk.dma("pool", Wglu[:], dr["w_glu"][l].rearrange("(k p) n -> p k n", p=128), [], [Wglu])
            tk.dma("sp", r1[:], dr["rows1"][l:l + 1, :].partition_broadcast(128), [], [r1])
            tk.dma("sp", cw[:], dr["convw"][:, l, :, :], [], [cw])
            tk.dma("sp", cbt[:], dr["convb"][:, l, :], [], [cbt])
            tk.dma("sp", ync[:], dr["ynormc"][:, l, :], [], [ync])
            dtb_bc = r1[:, 0:16]
            alog_bc = r1[:, 16:32]
            dssd_bc = r1[:, 32:48]
            bglu_bc = r1[:, 48:560]
            s5d_bc = r1[:, 560:1072]
            self.act(abc[:], alog_bc, AF.Exp, [r1], [abc])
            self.ts("dve", abc[:], abc[:], -1.0, None, ALU.mult, None, [abc], [abc])
            self.ts("dve", A1c[:], adac[:, l, 8:16], 1.0, None, ALU.add, None, [adac], [A1c])
            self.tt("dve", A1c[:], A1c[:], self.n1t[:, l, :], ALU.mult, [A1c, self.n1t], [A1c])
            shc = adac[:, l, 0:8]
            with ExitStack() as PP:
                r2 = sb(PP, "r2", [128, RW2])
                Tm = [sb(PP, f"Tm{i}", [128, 2048]) for i in range(6)]
                B1 = sb(PP, "B1", [128, 16, 128])
                B2 = sb(PP, "B2", [128, 16, 128])
                lct = sb(PP, "lct", [128, 3, 16])
                sc = [sb(PP, f"sc{i}", [128, 16]) for i in range(3)]
                C1 = sb(PP, "C1", [128, 16, 32])
                C2 = sb(PP, "C2", [128, 16, 32])
                self.dall = sb(PP, "dall", [128, 4, 128])
                gbc = sb(PP, "gbc", [128, D])
                tk.dma("sp", r2[:], dr["rows2"][l:l + 1, :].partition_broadcast(128), [], [r2])
                tk.dma("sp", B1[:], dr["bblk"][l, 0], [], [B1])
                tk.dma("act", B2[:], dr["bblk"][l, 1], [], [B2])
                tk.dma("sp", lct[:], dr["lamc"][:, l, :, :], [], [lct])
                tk.dma("sp", C1[:], dr["cblk"][l, 0], [], [C1])
                tk.dma("sp", C2[:], dr["cblk"][l, 1], [], [C2])
                ldt = r2[:, 0:32]
                lr = r2[:, 32:2080]
                li = r2[:, 2080:4128]
                T1, T2, T3, T4, T5, T6 = Tm
                v3 = lambda ap: ap.rearrange("p (g q) -> p g q", g=32)
                v16 = lambda ap: ap.rearrange("p (g q) -> p g q", g=16)
                stepR = sc[0]
                stR = T6[:, 0:32]
                self.act(stR, ldt, AF.Exp, [r2], [T6])
                stb = stR.unsqueeze(2).to_broadcast([128, 32, 64])
                self.tt("dve", v3(T1[:]), v3(lr), stb, ALU.mult, [r2, T6], [T1])
                self.tt("dve", v3(T2[:]), v3(li), stb, ALU.mult, [r2, T6], [T2])
                self.act(T3[:], T1[:], AF.Exp, [T1], [T3])
                self.rangered("dve", (T4[:], T4), T2[:], (T6[:], T6), [T2])
                self.act(T5[:], T4[:], AF.Sin, [T4], [T5])
                self.act(T6[:], T4[:], AF.Sin, [T4], [T6], scale=0.5)
                self.tt("dve", T6[:], T6[:], T6[:], ALU.mult, [T6], [T6])
                self.ts("dve", T6[:], T6[:], -2.0, 1.0, ALU.mult, ALU.add, [T6], [T6])
                self.tt("dve", T6[:], T3[:], T6[:], ALU.mult, [T3, T6], [T6])
                self.tt("dve", T5[:], T3[:], T5[:], ALU.mult, [T3, T5], [T5])
                self.tt("dve", T3[:], lr, lr, ALU.mult, [r2, T5, T6], [T3])
                self.tt("dve", T4[:], li, li, ALU.mult, [r2], [T4])
                self.tt("dve", T3[:], T3[:], T4[:], ALU.add, [T3, T4], [T3])
                tk.op("dve", lambda e: e.reciprocal(out=T3[:], in_=T3[:]), [T3], [T3])
                self.ts("dve", T6[:], T6[:], -1.0, None, ALU.add, None, [T6], [T6])
                self.tt("dve", T4[:], T6[:], lr, ALU.mult, [T6, r2], [T4])
                self.tt("dve", T1[:], T5[:], li, ALU.mult, [T5, r2], [T1])
                self.tt("dve", T4[:], T4[:], T1[:], ALU.add, [T4, T1], [T4])
                self.tt("dve", T4[:], T4[:], T3[:], ALU.mult, [T4, T3], [T4])
                self.tt("dve", T1[:], T5[:], lr, ALU.mult, [T5, r2, T4], [T1])
                self.tt("dve", T2[:], T6[:], li, ALU.mult, [T6, r2], [T2])
                self.tt("dve", T1[:], T1[:], T2[:], ALU.subtract, [T1, T2], [T1])
                self.tt("dve", T1[:], T1[:], T3[:], ALU.mult, [T1, T3], [T1])
                cre, cim = v16(T4[:]), v16(T1[:])
                self.tt("dve", v16(T2[:]), cre, B1[:], ALU.mult, [T4, B1], [T2])
                self.tt("dve", v16(T5[:]), cim, B2[:], ALU.mult, [T1, B2], [T5])
                self.tt("dve", Bb[:, :, 0, :], v16(T2[:]), v16(T5[:]), ALU.subtract, [T2, T5], [Bb])
                self.tt("dve", v16(T2[:]), cre, B2[:], ALU.mult, [T4, B2, Bb], [T2])
                self.tt("dve", v16(T5[:]), cim, B1[:], ALU.mult, [T1, B1, Bb], [T5])
                self.tt("dve", Bb[:, :, 1, :], v16(T2[:]), v16(T5[:]), ALU.add, [T2, T5], [Bb])
                stC, lrsC, lisC = sc
                self.act(stC[:], lct[:, 2, :], AF.Exp, [lct], [stC])
                self.tt("dve", lrsC[:], lct[:, 0, :], stC[:], ALU.mult, [lct, stC], [lrsC])
                self.tt("dve", lisC[:], lct[:, 1, :], stC[:], ALU.mult, [lct, stC], [lisC])
                self.act(mcol[:], lrsC[:], AF.Exp, [lrsC], [mcol])
                self.tt("dve", v16(T1[:]), lisC[:].unsqueeze(2).to_broadcast([128, 16, 128]),
                        self.iotar.unsqueeze(1).to_broadcast([128, 16, 128]), ALU.mult, [lisC, self.cstt], [T1])
                self.rangered("dve", (T2[:], T2), T1[:], (T3[:], T3), [T1])
                self.act(Qs[:].rearrange("p g q -> p (g q)"), T2[:], AF.Sin, [T2], [Qs])
                self.act(T3[:], T2[:], AF.Sin, [T2], [T3], scale=0.5)
                self.tt("dve", T3[:], T3[:], T3[:], ALU.mult, [T3], [T3])
                self.ts("dve", Qc[:].rearrange("p g q -> p (g q)"), T3[:], -2.0, 1.0, ALU.mult, ALU.add, [T3], [Qc])
                self.cp("dve", Cb[:, :, 0, :], C1[:], [C1], [Cb])
                self.ts("dve", Cb[:, :, 1, :], C2[:], -1.0, None, ALU.mult, None, [C2], [Cb])
                self.bcast_cols(adac[:, l, 16:24], gbc, [adac])
                for h in range(2):
                    self.tt("dve", Wout[:, :, h * 512:(h + 1) * 512], Wout[:, :, h * 512:(h + 1) * 512],
                            gbc[:, h * 512:(h + 1) * 512].unsqueeze(1).to_broadcast([128, 12, 512]), ALU.mult, [Wout, gbc], [Wout])
                tk.barrier()
            with ExitStack() as WK:
                xt2 = [sb(WK, "xta", [128, D]), sb(WK, "xtb", [128, D])]
                xnb = sb(WK, "xnb", [128, D], BF16)
                hT = sb(WK, "hT", [128, 8, 128], BF16)
                zs = sb(WK, "zs", [128, D])
                cin = sb(WK, "cin", [128, 12, 131])
                cacc = sb(WK, "cacc", [128, 2, 128])
                cout = sb(WK, "cout", [128, 8, 128])
                BT = sb(WK, "BT", [128, 2, 128], BF16)
                CT = sb(WK, "CT", [128, 2, 128], BF16)
                xs = sb(WK, "xs", [128, D])
                xdb = sb(WK, "xdb", [128, D], BF16)
                xddb = sb(WK, "xddb", [128, D], BF16)
                Btok = sb(WK, "Btok", [128, 256], BF16)
                sm = sb(WK, "sm", [128, 10, 16])
                st1 = sb(WK, "st1", [128, 8])
                stF = sb(WK, "stF", [128, 4])
                stB = sb(WK, "stB", [128, 4])
                seg = sb(WK, "seg", [128, 4, 128])
                CBs = sb(WK, "CBs", [128, 2, 128])
                MT = sb(WK, "MT", [128, 16, 128], BF16)
                H = sb(WK, "H", [128, D])
                Hb = sb(WK, "Hb", [128, D], BF16)
                yv = sb(WK, "yv", [128, D])
                tA = sb(WK, "tA", [128, D])
                ynb = sb(WK, "ynb", [128, D], BF16)
                ycT = sb(WK, "ycT", [128, 12, 128], BF16)
                uT = sb(WK, "uT", [128, 4, 128], BF16)
                du = sb(WK, "du", [128, 512])
                bus = sb(WK, "bus", [128, 4, 2, 128])
                t1 = sb(WK, "t1", [128, 4, 128])
                t2 = sb(WK, "t2", [128, 4, 128])
                rin = sb(WK, "rin", [128, 4, 2, 128])
                rr = sb(WK, "rr", [128, 4, 2, 128])
                sT = sb(WK, "sT", [128, 4, 2, 128], BF16)
                carry = sb(WK, "carry", [128, 16, 2])
                y5 = sb(WK, "y5", [128, 512])
                q5 = sb(WK, "q5", [128, 512])
                yg = sb(WK, "yg", [128, 512])
                ygb = sb(WK, "ygb", [128, 512], BF16)
                ygT = sb(WK, "ygT", [128, 4, 128], BF16)
                tk.op("pool", lambda e: e.memset(cin[:], 0.0), [], [cin])
                tk.op("pool", lambda e: e.memset(H[:], 0.0), [], [H])
                tk.op("pool", lambda e: e.memset(Hb[:], 0.0), [], [Hb])
                tk.op("pool", lambda e: e.memset(carry[:], 0.0), [], [carry])
                dt_, ad_, acs_, ea_, ds_, dtds_, cd_, tm_ = [sm[:, i, :] for i in range(8)]
                psb = lambda b: ps[:, b, :].bitcast(BF16)

                t3 = sb(WK, "t3", [128, 4, 128])
                t4 = sb(WK, "t4", [128, 4, 128])
                v16h = lambda ap: ap.rearrange("p (h q) -> p h q", h=16)
                ACTI = AF.Identity

                def front_steps(t):
                    S = []
                    xt = xt2[t % 2]
                    r0 = t * 128

                    def f1():
                        tk.dma("sp", xt[:], xsrc[r0:r0 + 128, :], [self.XD[t]], [xt])
                        self.act(xnb[:], xt[:], AF.Square, [xt], [xnb, stF], accum=stF[:, 0:1])
                        self.rstd(stF[:, 0:1], stF[:, 1:2], stF[:, 2:3], D, [stF], [stF])
                        self.act(xnb[:], xt[:], AF.Copy, [xt, stF], [xnb], scale=stF[:, 2:3])
                        bk = self.pb()
                        pv = psb(bk).rearrange("p (k q) -> p k q", k=8)
                        self.trs([(pv[:, k, :], xnb[:, k * 128:(k + 1) * 128]) for k in range(8)], self.identb[:], [xnb, self.identb], [PB[bk]])
                        for k in range(8):
                            self.act(hT[:, k, :], pv[:, k, :], ACTI, [PB[bk], A1c, adac], [hT], scale=A1c[:, k:k + 1], bias=shc[:, k:k + 1])
                    S.append(f1)

                    def f2(h):
                        bk = self.pb()
                        self.mm(ps[:, bk, :], [(hT[:, k, :], Win[:, k, h * 512:(h + 1) * 512]) for k in range(8)], [hT, Win], [PB[bk]])
                        self.act(zs[:, h * 512:(h + 1) * 512], ps[:, bk, :], AF.Silu, [PB[bk]], [zs])
                    S.append(lambda: f2(0))
                    S.append(lambda: f2(1))

                    def f3():
                        bk = self.pb()
                        self.mm(ps[:, bk, 0:16], [(hT[:, k, :], Win[:, k, 2560:2576]) for k in range(8)], [hT, Win], [PB[bk]])
                        self.tt("dve", tm_, ps[:, bk, 0:16], dtb_bc, ALU.add, [PB[bk], r1], [sm])
                        self.act(tm_, tm_, AF.Exp, [sm], [sm])
                        self.act(dt_, tm_, AF.Ln, [sm], [sm], bias=1.0)
                        self.tt("dve", ad_, dt_, abc[:], ALU.mult, [sm, abc], [sm])
                        bk = self.pb()
                        self.mm(ps[:, bk, :], [(hT[:, k, :], Win[:, k, 2576:3088]) for k in range(8)], [hT, Win], [PB[bk]])
                        self.tt("dve", du[:], ps[:, bk, :], s5d_bc, ALU.mult, [PB[bk], r1], [du])
                    S.append(f3)

                    def f4(g3):
                        bk = self.pb()
                        for cc in range(4):
                            c = g3 * 4 + cc
                            self.mm(ps[:, bk, cc * 128:(cc + 1) * 128],
                                    [(Win[:, k, 1024 + c * 128:1024 + (c + 1) * 128], hT[:, k, :]) for k in range(8)], [hT, Win], [PB[bk]])
                        self.cp("act", cin[:, g3 * 4:(g3 + 1) * 4, 3:131], ps[:, bk, :].rearrange("p (c q) -> p c q", c=4), [PB[bk]], [cin])
                    for g3 in range(3):
                        S.append(lambda g3=g3: f4(g3))

                    def f5():
                        bk = self.pb()
                        for c in range(4):
                            self.mm(ps[:, bk, c * 128:(c + 1) * 128],
                                    [(Win[:, k, 2576 + c * 128:2576 + (c + 1) * 128], hT[:, k, :]) for k in range(8)], [hT, Win], [PB[bk]])
                        self.cp("act", uT[:], ps[:, bk, :].rearrange("p (c q) -> p c q", c=4), [PB[bk]], [uT])
                    S.append(f5)

                    def f6(c):
                        ca = cacc[:, c % 2, :]
                        self.act(ca, cin[:, c, 0:128], ACTI, [cin, cw, cbt], [cacc], scale=cw[:, c, 0:1], bias=cbt[:, c:c + 1])
                        for k in range(1, 4):
                            self.stt(ca, cin[:, c, k:k + 128], cw[:, c, k:k + 1], ca, ALU.mult, ALU.add, [cin, cw, cacc], [cacc])
                        if c < 8:
                            self.act(cout[:, c, :], ca, AF.Silu, [cacc], [cout])
                        elif c < 10:
                            self.act(BT[:, c - 8, :], ca, AF.Silu, [cacc], [BT])
                        else:
                            self.act(CT[:, c - 10, :], ca, AF.Silu, [cacc], [CT])
                    for c in range(12):
                        S.append(lambda c=c: f6(c))

                    def f7():
                        self.cp("pool", cin[:, :, 0:3], cin[:, :, 128:131], [cin], [cin])
                        for h in range(2):
                            bk = self.pb()
                            self.trs([(ps[:, bk, cc * 128:(cc + 1) * 128], cout[:, h * 4 + cc, :]) for cc in range(4)], self.identf, [cout, self.cstt], [PB[bk]])
                            self.cp("act", xs[:, h * 512:(h + 1) * 512], ps[:, bk, :], [PB[bk]], [xs])
                        bk = self.pb()
                        pvb = psb(bk)
                        self.trs([(pvb[:, g * 128:(g + 1) * 128], BT[:, g, :]) for g in range(2)], self.identb[:], [BT, self.identb], [PB[bk]])
                        self.cp("dve", Btok[:], pvb[:, 0:256], [PB[bk]], [Btok])
                    S.append(f7)
                    return S

                def ssd_steps(t):
                    S = []

                    def s1():
                        bk = self.pb()
                        self.mm(ps[:, bk, 0:16], [(self.triuf, ad_)], [self.cstt, sm], [PB[bk]])
                        self.cp("dve", acs_, ps[:, bk, 0:16], [PB[bk]], [sm])
                        self.act(ea_, acs_, AF.Exp, [sm], [sm])
                        bkc = self.pb()
                        self.mms([(ps[:, bkc, g * 128:(g + 1) * 128], BT[:, g, :], CT[:, g, :]) for g in range(2)], [BT, CT], [PB[bkc]])
                        self.cp("act", CBs[:], ps[:, bkc, 0:256].rearrange("p (g q) -> p g q", g=2), [PB[bkc]], [CBs])
                    S.append(s1)

                    def sq(q):
                        h0 = q * 4
                        g = h0 // 8
                        bk = self.pb()
                        self.mms([(ps[:, bk, hh * 128:(hh + 1) * 128], ad_[:, h0 + hh:h0 + hh + 1].to_broadcast([128, 128]), self.triuf) for hh in range(4)],
                                 [sm, self.cstt], [PB[bk]])
                        pv4 = ps[:, bk, :].rearrange("p (h q) -> p h q", h=4)
                        self.act(cd_[:, h0:h0 + 4], pv4[:, :, 127], AF.Exp, [PB[bk]], [sm])
                        self.tt("dve", tm_[:, h0:h0 + 4], pv4[:, :, 127], acs_[:, h0:h0 + 4], ALU.subtract, [PB[bk], sm], [sm])
                        for hh in range(4):
                            self.stt(seg[:, hh, :], pv4[:, hh, :], acs_[:, h0 + hh:h0 + hh + 1], self.maskb, ALU.subtract, ALU.add, [PB[bk], sm, self.cstt], [seg])
                        self.act(seg[:], seg[:], AF.Exp, [seg], [seg])
                        self.tt("dve", MT[:, h0:h0 + 4, :], seg[:], CBs[:, g:g + 1, :].to_broadcast([128, 4, 128]), ALU.mult, [seg, CBs], [MT])
                    for q in range(4):
                        S.append(lambda q=q: sq(q))

                    def s7():
                        self.act(ds_, tm_, AF.Exp, [sm], [sm])
                        self.tt("dve", dtds_, dt_, ds_, ALU.mult, [sm], [sm])
                        self.tt("dve", v16h(xdb[:]), v16h(xs[:]), dt_.unsqueeze(2).to_broadcast([128, 16, 64]), ALU.mult, [xs, sm], [xdb])
                        self.tt("dve", v16h(xddb[:]), v16h(xs[:]), dtds_.unsqueeze(2).to_broadcast([128, 16, 64]), ALU.mult, [xs, sm], [xddb])
                    S.append(s7)

                    def sy(g):
                        bko = self.pb()
                        self.mm(ps[:, bko, :], [(CT[:, g, :], Hb[:, g * 512:(g + 1) * 512])], [CT, Hb], [PB[bko]])
                        self.tt("dve", v16h(tA[:])[:, g * 8:(g + 1) * 8, :], ps[:, bko, :].rearrange("p (h q) -> p h q", h=8),
                                ea_[:, g * 8:(g + 1) * 8].unsqueeze(2).to_broadcast([128, 8, 64]), ALU.mult, [PB[bko], sm], [tA])
                        bkd = self.pb()
                        self.mms([(ps[:, bkd, hh * 64:(hh + 1) * 64], MT[:, g * 8 + hh, :], xdb[:, (g * 8 + hh) * 64:(g * 8 + hh + 1) * 64]) for hh in range(8)],
                                 [MT, xdb], [PB[bkd]])
                        self.tt("dve", yv[:, g * 512:(g + 1) * 512], ps[:, bkd, :], tA[:, g * 512:(g + 1) * 512], ALU.add, [PB[bkd], tA], [yv])
                    S.append(lambda: sy(0))
                    S.append(lambda: sy(1))

                    def s10():
                        self.tt("dve", v16h(H[:]), v16h(H[:]), cd_.unsqueeze(2).to_broadcast([128, 16, 64]), ALU.mult, [H, sm], [H])
                        for g in range(2):
                            bk = self.pb()
                            self.mm(ps[:, bk, :], [(Btok[:, g * 128:(g + 1) * 128], xddb[:, g * 512:(g + 1) * 512])], [Btok, xddb], [PB[bk]])
                            self.tt("dve", H[:, g * 512:(g + 1) * 512], ps[:, bk, :], H[:, g * 512:(g + 1) * 512], ALU.add, [PB[bk], H], [H])
                        self.cp("act", Hb[:], H[:], [H], [Hb])
                    S.append(s10)

                    def s11():
                        self.tt("dve", v16h(tA[:]), v16h(xs[:]), dssd_bc.unsqueeze(2).to_broadcast([128, 16, 64]), ALU.mult, [xs, r1], [tA])
                        self.tt("dve", yv[:], yv[:], tA[:], ALU.add, [yv, tA], [yv])
                        self.tt("dve", yv[:], yv[:], zs[:], ALU.mult, [yv, zs], [yv])
                    S.append(s11)

                    def s12():
                        for g in range(2):
                            self.act(tA[:, g * 512:(g + 1) * 512], yv[:, g * 512:(g + 1) * 512], AF.Square, [yv], [tA, st1], accum=st1[:, 3 + g:4 + g])
                        self.rstd(st1[:, 3:5], st1[:, 5:7], st1[:, 3:5], 512, [st1], [st1])
                        for g in range(2):
                            self.act(ynb[:, g * 512:(g + 1) * 512], yv[:, g * 512:(g + 1) * 512], AF.Copy, [yv, st1], [ynb], scale=st1[:, 3 + g:4 + g])
                        bk = self.pb()
                        pv = psb(bk).rearrange("p (k q) -> p k q", k=8)
                        self.trs([(pv[:, k, :], ynb[:, k * 128:(k + 1) * 128]) for k in range(8)], self.identb[:], [ynb, self.identb], [PB[bk]])
                        self.tt("dve", ycT[:, 0:8, :], pv, ync[:, 0:8].unsqueeze(2).to_broadcast([128, 8, 128]), ALU.mult, [PB[bk], ync], [ycT])
                    S.append(s12)
                    return S

                def s5_steps(t, bky):
                    S = []

                    bub = {}

                    def bu(c):
                        gp0 = c * 4
                        for hb in range(2):
                            bk = self.pb()
                            bub[(c, hb)] = bk
                            self.mms([(ps[:, bk, (gg * 2 + ri) * 128:(gg * 2 + ri + 1) * 128], Bb[:, gp0 + hb * 2 + gg, ri, :], uT[:, c, :])
                                      for gg in range(2) for ri in range(2)], [Bb, uT], [PB[bk]])

                    def pre(c):
                        gp0 = c * 4
                        qc = Qc[:, gp0:gp0 + 4, :]
                        qs = Qs[:, gp0:gp0 + 4, :]
                        for hb in range(2):
                            bk = bub[(c, hb)]
                            pvv = ps[:, bk, :].rearrange("p (g r q) -> p g r q", g=2, r=2)
                            bre, bim = pvv[:, :, 0, :], pvv[:, :, 1, :]
                            qch = Qc[:, gp0 + hb * 2:gp0 + hb * 2 + 2, :]
                            qsh = Qs[:, gp0 + hb * 2:gp0 + hb * 2 + 2, :]
                            hs = slice(hb * 2, hb * 2 + 2)
                            self.tt("dve", t1[:, hs, :], qch, bre, ALU.mult, [Qc, PB[bk]], [t1])
                            self.tt("dve", t2[:, hs, :], qsh, bim, ALU.mult, [Qs, PB[bk]], [t2])
                            self.tt("dve", t3[:, hs, :], qch, bim, ALU.mult, [Qc, PB[bk]], [t3])
                            self.tt("dve", t4[:, hs, :], qsh, bre, ALU.mult, [Qs, PB[bk]], [t4])
                        self.tt("dve", rin[:, :, 0, :], t1[:], t2[:], ALU.add, [t1, t2], [rin])
                        self.tt("dve", rin[:, :, 1, :], t3[:], t4[:], ALU.subtract, [t3, t4], [rin])

                    def scan(c):
                        gp0 = c * 4
                        for gg in range(4):
                            for ri in range(2):
                                gp = gp0 + gg
                                tk.op("dve", lambda e, gg=gg, ri=ri, gp=gp: e.tensor_tensor_scan(
                                    out=rr[:, gg, ri, :], data0=mcol[:, gp:gp + 1].to_broadcast([128, 128]), data1=rin[:, gg, ri, :],
                                    initial=carry[:, gp, ri:ri + 1], op0=ALU.mult, op1=ALU.add), [mcol, rin, carry], [rr])

                    def post(c):
                        gp0 = c * 4
                        qc = Qc[:, gp0:gp0 + 4, :]
                        qs = Qs[:, gp0:gp0 + 4, :]
                        rre, rim = rr[:, :, 0, :], rr[:, :, 1, :]
                        self.tt("dve", t1[:], qc, rre, ALU.mult, [Qc, rr], [t1])
                        self.tt("dve", t2[:], qs, rim, ALU.mult, [Qs, rr], [t2])
                        self.tt("dve", t3[:], qc, rim, ALU.mult, [Qc, rr], [t3])
                        self.tt("dve", t4[:], qs, rre, ALU.mult, [Qs, rr], [t4])
                        self.tt("dve", sT[:, :, 0, :], t1[:], t2[:], ALU.subtract, [t1, t2], [sT])
                        self.tt("dve", sT[:, :, 1, :], t3[:], t4[:], ALU.add, [t3, t4], [sT])
                        self.tt("dve", carry[:, gp0:gp0 + 4, 0], t1[:, :, 127], t2[:, :, 127], ALU.subtract, [t1, t2], [carry])
                        self.tt("dve", carry[:, gp0:gp0 + 4, 1], t3[:, :, 127], t4[:, :, 127], ALU.add, [t3, t4], [carry])
                        for gg in range(4):
                            gp = gp0 + gg
                            self.mm(ps[:, bky, gp * 32:(gp + 1) * 32], [(sT[:, gg, 0, :], Cb[:, gp, 0, :]), (sT[:, gg, 1, :], Cb[:, gp, 1, :])], [sT, Cb], [PB[bky]])
                    for c in range(4):
                        S.append(lambda c=c: bu(c))
                        S.append(lambda c=c: pre(c))
                        S.append(lambda c=c: scan(c))
                        S.append(lambda c=c: post(c))
                    return S

                def back_steps(t, bky):
                    S = []
                    xt = xt2[t % 2]
                    r0 = t * 128

                    def b1():
                        self.tt("dve", q5[:], y5[:], y5[:], ALU.mult, [y5], [q5])
                        self.ts("dve", q5[:], q5[:], 0.044715, 1.0, ALU.mult, ALU.add, [q5], [q5])
                        self.tt("dve", q5[:], q5[:], y5[:], ALU.mult, [q5, y5], [q5])
                        self.act(q5[:], q5[:], AF.Sigmoid, [q5], [q5], scale=1.5957691216057308)
                    S.append(b1)

                    def b2():
                        self.tt("dve", yg[:], y5[:], q5[:], ALU.mult, [y5, q5], [yg])
                        self.cp("act", ygb[:], yg[:], [yg], [ygb])
                        bk = self.pb()
                        pvb = psb(bk)
                        self.trs([(pvb[:, k * 128:(k + 1) * 128], ygb[:, k * 128:(k + 1) * 128]) for k in range(4)], self.identb[:], [ygb, self.identb], [PB[bk]])
                        self.cp("act", ygT[:], pvb[:, 0:512].rearrange("p (k q) -> p k q", k=4), [PB[bk]], [ygT])
                    S.append(b2)

                    def b3():
                        bk = self.pb()
                        self.mm(ps[:, bk, :], [(ygT[:, k, :], Wglu[:, k, :]) for k in range(4)], [ygT, Wglu], [PB[bk]])
                        self.tt("dve", q5[:], ps[:, bk, :], bglu_bc, ALU.add, [PB[bk], r1], [q5])
                        self.act(q5[:], q5[:], AF.Sigmoid, [q5], [q5])
                    S.append(b3)

                    def b4():
                        self.tt("dve", yg[:], yg[:], q5[:], ALU.mult, [yg, q5], [yg])
                        self.act(q5[:], yg[:], AF.Square, [yg], [q5, stB], accum=stB[:, 0:1])
                        self.rstd(stB[:, 0:1], stB[:, 1:2], stB[:, 2:3], 512, [stB], [stB])
                        self.act(ygb[:], yg[:], AF.Copy, [yg, stB], [ygb], scale=stB[:, 2:3])
                    S.append(b4)

                    def b5():
                        bk = self.pb()
                        pvb = psb(bk)
                        self.trs([(pvb[:, k * 128:(k + 1) * 128], ygb[:, k * 128:(k + 1) * 128]) for k in range(4)], self.identb[:], [ygb, self.identb], [PB[bk]])
                        self.tt("dve", ycT[:, 8:12, :], pvb[:, 0:512].rearrange("p (k q) -> p k q", k=4),
                                ync[:, 8:12].unsqueeze(2).to_broadcast([128, 4, 128]), ALU.mult, [PB[bk], ync], [ycT])
                    S.append(b5)

                    def b6(h):
                        bk = self.pb()
                        self.mm(ps[:, bk, :], [(ycT[:, k, :], Wout[:, k, h * 512:(h + 1) * 512]) for k in range(12)], [ycT, Wout], [PB[bk]])
                        self.tt("dve", tA[:, h * 512:(h + 1) * 512], ps[:, bk, :], xt[:, h * 512:(h + 1) * 512], ALU.add, [PB[bk], xt], [tA])
                    S.append(lambda: b6(0))

                    def b7():
                        b6(1)
                        tk.dma("sp", xdst[r0:r0 + 128, :], tA[:], [tA], [self.XD[t]])
                    S.append(b7)
                    return S

                def interleave(A, Bs, ratio=1):
                    ia = ib = 0
                    while ia < len(A) or ib < len(Bs):
                        for _ in range(ratio):
                            if ib < len(Bs):
                                Bs[ib]()
                                ib += 1
                        if ia < len(A):
                            A[ia]()
                            ia += 1

                for f in front_steps(0):
                    f()
                for t in range(NT):
                    bky = 7
                    nf = front_steps(t + 1) if t + 1 < NT else []
                    A, Bs = ssd_steps(t), s5_steps(t, bky)
                    if nf:
                        Bs = Bs[:8] + [nf[0]] + Bs[8:]
                    interleave(A, Bs)
                    self.tt("dve", y5[:], ps[:, bky, :], du[:], ALU.add, [PB[bky], du], [y5])
                    interleave(back_steps(t, bky), nf[1:], ratio=3)
                tk.barrier()
            tk.barrier()

    def moe_phase(self, l, xsrc, xdst, fnw, last):
        nc, tk, ps, PB = self.nc, self.tk, self.ps, self.PB
        sb = self.sb
        dr = self.dr
        adac = self.adac
        OUTR = self.YD if last else self.XD
        with ExitStack() as E:
            xall = [sb(E, f"xall{t}", [128, D]) for t in range(NT)]
            h2T = [sb(E, f"h2T{m}", [128, 8, 512], BF16) for m in range(4)]
            Wg = [sb(E, f"Wg{i}", [128, 8, 512], BF16) for i in range(2)]
            Wu = [sb(E, f"Wu{i}", [128, 8, 512], BF16) for i in range(2)]
            Wd = [sb(E, f"Wd{i}", [128, 4, D], BF16) for i in range(2)]
            actb = [sb(E, f"actb{i}", [128, 4, 512], BF16) for i in range(2)]
            sg = [sb(E, f"sg{i}", [128, 512]) for i in range(2)]
            Gall = sb(E, "Gall", [128, NT, 32])
            gfbc = sb(E, "gfbc", [128, D])
            h2f = sb(E, "h2f", [128, 8, 128])
            xn = sb(E, "xn", [128, D])
            junk = sb(E, "junk", [128, D], BF16)
            Wr = sb(E, "Wr", [128, 8, 36])
            r1 = sb(E, "r1e", [128, RW1])
            A2c = sb(E, "A2c", [128, 8])
            st1 = sb(E, "st1e", [128, 8])
            LG = sb(E, "LG", [128, 36])
            rt = sb(E, "rt", [128, 6, 32])
            sm = sb(E, "sme", [128, 16])
            self.dall = sb(E, "dalle", [128, 4, 128])
            do_ada = (l + 1 < self.nl)
            if do_ada:
                wab = sb(E, "wab", [128, 8, 256])
                arw = sb(E, "arw", [1, 256])
                badl = sb(E, "badl", [128, 48])
                tk.dma("sp", badl[:], self.badac[:, l + 1, :], [], [badl])
            BK7 = PB[7]

            def ada_issue(i):
                src = self.w_ada[l + 1, :, i * 256:(i + 1) * 256].rearrange("(k p) n -> p k n", p=128)
                tk.dma("sp", wab[:], src, [], [wab])

            def ada_consume(i):
                bank = self.pb()
                self.mm(ps[0:1, bank, 0:256], [(self.cact[:, k:k + 1], wab[:, k, :]) for k in range(8)], [wab, self.cact], [PB[bank]])
                self.cp("act", arw[0:1, :], ps[0:1, bank, 0:256], [PB[bank]], [arw])
                self.mms([(ps[:, 7, 2 * i + jj:2 * i + jj + 1], arw[0:1, jj * 128:(jj + 1) * 128], self.onesf[0:1, 0:1]) for jj in range(2)],
                         [arw, self.onesf], [BK7])
            PBUF = [(xn, h2f, st1, LG, rt, sm),
                    (sb(E, "xn2", [128, D]), sb(E, "h2f2", [128, 8, 128]), sb(E, "st1e2", [128, 8]), sb(E, "LG2", [128, 36]),
                     sb(E, "rt2", [128, 6, 32]), sb(E, "sme2", [128, 16]))]
            tk.dma("sp", Wr[:], dr["wr"][l].rearrange("(k p) n -> p k n", p=128), [], [Wr])
            tk.dma("sp", r1[:], dr["rows1"][l:l + 1, :].partition_broadcast(128), [], [r1])
            brc = r1[:, 1072:1108]
            self.ts("dve", A2c[:], adac[:, l, 32:40], 1.0, None, ALU.add, None, [adac], [A2c])
            self.tt("dve", A2c[:], A2c[:], self.n2t[:, l, :], ALU.mult, [A2c, self.n2t], [A2c])
            shf = adac[:, l, 24:32]
            self.bcast_cols(adac[:, l, 40:48], gfbc, [adac])

            def load_expert(e):
                i = e % 2
                for k0 in range(0, 8, 4):
                    tk.dma("pool", Wg[i][:, k0:k0 + 4, :], dr["w_eg"][l, e, k0 * 128:(k0 + 4) * 128, :].rearrange("(k p) n -> p k n", p=128), [], [Wg[i]])
                    tk.dma("pool", Wu[i][:, k0:k0 + 4, :], dr["w_eu"][l, e, k0 * 128:(k0 + 4) * 128, :].rearrange("(k p) n -> p k n", p=128), [], [Wu[i]])
                for k0 in range(0, 4, 2):
                    tk.dma("pool", Wd[i][:, k0:k0 + 2, :], dr["w_ed"][l, e, k0 * 128:(k0 + 2) * 128, :].rearrange("(k p) n -> p k n", p=128), [], [Wd[i]])
                tk.op("pool", lambda en: en.tensor_tensor(out=Wd[i][:], in0=Wd[i][:], in1=gfbc[:].unsqueeze(1).to_broadcast([128, 4, D]), op=ALU.mult), [Wd[i], gfbc], [Wd[i]])

            GR = [Reg(f"g{t}") for t in range(NT)]

            def pro_steps(t):
                r0 = t * 128
                xa = xall[t]
                xn, h2f, st1, LG, rt, sm = PBUF[t % 2]

                def p1():
                    tk.dma("sp", xa[:], xsrc[r0:r0 + 128, :], [self.XD[t]], [xa])
                    self.act(junk[:], xa[:], AF.Square, [xa], [junk, st1], accum=st1[:, 0:1])
                    self.rstd(st1[:, 0:1], st1[:, 1:2], st1[:, 2:3], D, [st1], [st1])
                    self.ts("dve", xn[:], xa[:], st1[:, 2:3], None, ALU.mult, None, [xa, st1], [xn])

                def p2():
                    for h in range(2):
                        bk = self.pb()
                        self.trs([(ps[:, bk, cc * 128:(cc + 1) * 128], xn[:, (h * 4 + cc) * 128:(h * 4 + cc + 1) * 128]) for cc in range(4)], self.identf, [xn, self.cstt], [PB[bk]])
                        pv = ps[:, bk, :].rearrange("p (k q) -> p k q", k=4)
                        for cc in range(4):
                            k = h * 4 + cc
                            self.act(h2f[:, k, :], pv[:, cc, :], AF.Identity, [PB[bk], A2c, adac], [h2f], scale=A2c[:, k:k + 1], bias=shf[:, k:k + 1])
                    self.cp("dve", h2T[t // 4][:, :, (t % 4) * 128:(t % 4 + 1) * 128], h2f[:], [h2f], [h2T[t // 4]])

                def p3():
                    bk = self.pb()
                    self.mm(ps[:, bk, 0:36], [(h2f[:, k, :], Wr[:, k, :]) for k in range(8)], [h2f, Wr], [PB[bk]])
                    self.tt("dve", LG[:], ps[:, bk, 0:36], brc, ALU.add, [PB[bk], r1], [LG])
                    gl = LG[:, 0:4]
                    el = LG[:, 4:36]
                    gmax, ngmax, gsum, gw, m1, m2, d21, e2, den, g1, g2 = [sm[:, i:i + 1] for i in range(11)]
                    ohg, pen, eg = rt[:, 0, 0:4], rt[:, 0, 8:12], rt[:, 0, 16:20]
                    ME, oh1, ME2, oh2 = rt[:, 1, :], rt[:, 2, :], rt[:, 3, :], rt[:, 4, :]
                    V = "dve"
                    tk.op(V, lambda e: e.reduce_max(out=gmax, in_=gl, axis=AX.X), [LG], [sm])
                    self.ts(V, ohg, gl, gmax, None, ALU.is_equal, None, [LG, sm], [rt])
                    self.ts(V, ngmax, gmax, -1.0, None, ALU.mult, None, [sm], [sm])
                    self.act(eg, gl, AF.Exp, [LG, sm], [rt, sm], bias=ngmax, accum=gsum)
                    tk.op(V, lambda e: e.reciprocal(out=gw, in_=gsum), [sm], [sm])
                    self.ts(V, pen, ohg, -1.0, 1e30, ALU.add, ALU.mult, [rt], [rt])
                    self.tt(V, ME.rearrange("p (g q) -> p g q", g=4), el.rearrange("p (g q) -> p g q", g=4),
                            pen.unsqueeze(2).to_broadcast([128, 4, 8]), ALU.add, [LG, rt], [rt])
                    tk.op(V, lambda e: e.reduce_max(out=m1, in_=ME, axis=AX.X), [rt], [sm])
                    self.ts(V, oh1, ME, m1, None, ALU.is_equal, None, [rt, sm], [rt])
                    self.stt(ME2, oh1, -1e30, ME, ALU.mult, ALU.add, [rt], [rt])
                    tk.op(V, lambda e: e.reduce_max(out=m2, in_=ME2, axis=AX.X), [rt], [sm])
                    self.ts(V, oh2, ME2, m2, None, ALU.is_equal, None, [rt, sm], [rt])
                    self.tt(V, d21, m2, m1, ALU.subtract, [sm], [sm])
                    self.act(e2, d21, AF.Exp, [sm], [sm])
                    self.ts(V, den, e2, 1.0, None, ALU.add, None, [sm], [sm])
                    tk.op(V, lambda e: e.reciprocal(out=den, in_=den), [sm], [sm])
                    self.tt(V, g1, gw, den, ALU.mult, [sm], [sm])
                    self.tt(V, g2, g1, e2, ALU.mult, [sm], [sm])
                    self.ts(V, oh1, oh1, g1, None, ALU.mult, None, [rt, sm], [rt])
                    self.stt(Gall[:, t, :], oh2, g2, oh1, ALU.mult, ALU.add, [rt, sm], [GR[t]])
                return [p1, p2, p3]

            def epilogue(t):
                r0 = t * 128
                xa = xall[t]
                if last:
                    self.act(junk[:], xa[:], AF.Square, [xa], [junk, st1], accum=st1[:, 0:1])
                    self.rstd(st1[:, 0:1], st1[:, 1:2], st1[:, 2:3], D, [st1], [st1])
                    self.ts("dve", xn[:], xa[:], st1[:, 2:3], None, ALU.mult, None, [xa, st1], [xn])
                    self.tt("dve", xa[:], xn[:], gfbc[:], ALU.mult, [xn, gfbc], [xa])
                tk.dma("sp", xdst[r0:r0 + 128, :], xa[:], [xa], [OUTR[t]])

            load_expert(0)
            for e in range(32):
                i = e % 2
                if e + 1 < 32:
                    load_expert(e + 1)
                if last and e == 31:
                    tk.dma("sp", gfbc[:], fnw[0:1, :].partition_broadcast(128), [], [gfbc])
                if do_ada:
                    if 2 <= e <= 25:
                        ada_consume(e - 2)
                    if 1 <= e <= 24:
                        ada_issue(e - 1)
                    if e == 26:
                        self.tt("dve", adac[:, l + 1, :], ps[:, 7, 0:48], badl[:], ALU.add, [BK7, badl], [adac])
                for m in range(4):
                    PS = []
                    if e == 0:
                        if m == 0:
                            for pr in range(2):
                                sa, sb_ = pro_steps(2 * pr), pro_steps(2 * pr + 1)
                                for f in (sa[0], sb_[0], sa[1], sb_[1], sa[2], sb_[2]):
                                    f()
                        if m < 3:
                            t0_ = 4 * (m + 1)
                            for pr in range(2):
                                sa, sb_ = pro_steps(t0_ + 2 * pr), pro_steps(t0_ + 2 * pr + 1)
                                PS += [sa[0], sb_[0], sa[1], sb_[1], sa[2], sb_[2]]

                    def slot():
                        if PS:
                            PS.pop(0)()
                    ab = actb[m % 2]
                    for j in range(4):
                        slot()
                        bg = self.pb()
                        self.mm(ps[:, bg, :], [(Wg[i][:, k, j * 128:(j + 1) * 128], h2T[m][:, k, :]) for k in range(8)], [Wg[i], h2T[m]], [PB[bg]])
                        bu_ = self.pb()
                        self.mm(ps[:, bu_, :], [(Wu[i][:, k, j * 128:(j + 1) * 128], h2T[m][:, k, :]) for k in range(8)], [Wu[i], h2T[m]], [PB[bu_]])
                        s_ = sg[j % 2]
                        self.act(s_[:], ps[:, bg, :], AF.Silu, [PB[bg]], [s_])
                        self.tt("dve", ab[:, j, :], ps[:, bu_, :], s_[:], ALU.mult, [PB[bu_], s_], [ab])
                    for s4 in range(4):
                        t = m * 4 + s4
                        for h in range(2):
                            slot()
                            bk = self.pb()
                            self.mm(ps[:, bk, :], [(ab[:, j, s4 * 128:(s4 + 1) * 128], Wd[i][:, j, h * 512:(h + 1) * 512]) for j in range(4)], [ab, Wd[i]], [PB[bk]])
                            self.stt(xall[t][:, h * 512:(h + 1) * 512], ps[:, bk, :], Gall[:, t, e:e + 1], xall[t][:, h * 512:(h + 1) * 512],
                                     ALU.mult, ALU.add, [PB[bk], GR[t], xall[t]], [xall[t]])
                    if e == 31:
                        for t in range(4 * m, 4 * m + 4):
                            epilogue(t)
            tk.barrier()


def _col(v, nchunk):
    return np.ascontiguousarray(v.reshape(nchunk, 128).T)


def prep_shared(inp, nl=DEPTH):
    f = np.float32
    L = DEPTH
    sh = {}
    sh["w_ada"] = np.ascontiguousarray(inp["w_ada"], dtype=f)
    sh["badac"] = np.ascontiguousarray(np.stack([_col(inp["b_ada"][l], 48) for l in range(L)], axis=1), dtype=f)
    sh["n1c"] = np.ascontiguousarray(np.stack([_col(inp["norm1_w"][l], 8) for l in range(L)], axis=1), dtype=f)
    sh["n2c"] = np.ascontiguousarray(np.stack([_col(inp["norm2_w"][l], 8) for l in range(L)], axis=1), dtype=f)
    sh["w_in"] = np.ascontiguousarray(inp["w_in"], dtype=f)
    sh["w_out"] = np.ascontiguousarray(inp["w_out"], dtype=f)
    sh["w_glu"] = np.ascontiguousarray(inp["w_glu"], dtype=f)
    cw = np.zeros((128, L, 12, 4), f)
    cb = np.zeros((128, L, 12), f)
    yn = np.zeros((128, L, 12), f)
    for l in range(L):
        cw[:, l] = inp["conv_w"][l].T.reshape(12, 128, 4).transpose(1, 0, 2)
        cb[:, l] = _col(inp["conv_b"][l], 12)
        yn[:, l] = _col(np.concatenate([inp["ssd_norm_w"][l], inp["s5_norm_w"][l]]), 12)
    sh["convw"], sh["convb"], sh["ynormc"] = cw, cb, yn
    sh["rows1"] = np.ascontiguousarray(np.concatenate(
        [inp["dt_bias"], inp["a_log"], inp["d_ssd"], inp["b_glu"], inp["s5_d"], inp["b_rg"], inp["b_re"]], axis=1), dtype=f)
    sh["rows2"] = np.ascontiguousarray(np.concatenate(
        [inp["s5_log_dt"], inp["s5_lam_re"].reshape(L, -1), inp["s5_lam_im"].reshape(L, -1)], axis=1), dtype=f)
    sh["fnw"] = np.ascontiguousarray(inp["final_norm_w"].reshape(1, D), dtype=f)
    lamc = np.zeros((128, L, 3, 16), f)
    bblk = np.zeros((L, 2, 128, 16, 128), f)
    cblk = np.zeros((L, 2, 128, 16, 32), f)
    for l in range(L):
        for gp in range(16):
            for g2 in range(2):
                g = 2 * gp + g2
                lamc[g2 * 64:(g2 + 1) * 64, l, 0, gp] = inp["s5_lam_re"][l, g]
                lamc[g2 * 64:(g2 + 1) * 64, l, 1, gp] = inp["s5_lam_im"][l, g]
                lamc[g2 * 64:(g2 + 1) * 64, l, 2, gp] = inp["s5_log_dt"][l, g]
                gl = (gp % 4) * 2 + g2
                for ri, nm in enumerate(("s5_b_re", "s5_b_im")):
                    bblk[l, ri, gl * 16:(gl + 1) * 16, gp, g2 * 64:(g2 + 1) * 64] = inp[nm][l, g].T
                for ri, nm in enumerate(("s5_c_re", "s5_c_im")):
                    cblk[l, ri, g2 * 64:(g2 + 1) * 64, gp, g2 * 16:(g2 + 1) * 16] = inp[nm][l, g].T
    sh["lamc"], sh["bblk"], sh["cblk"] = lamc, bblk, cblk
    sh["wr"] = np.ascontiguousarray(np.concatenate([inp["w_rg"], inp["w_re"]], axis=2), dtype=f)
    sh["w_eg"] = np.ascontiguousarray(inp["w_eg"], dtype=f)
    sh["w_eu"] = np.ascontiguousarray(inp["w_eu"], dtype=f)
    sh["w_ed"] = np.ascontiguousarray(inp["w_ed"], dtype=f)
    cst = np.zeros((128, 4, 128), f)
    cst[:, 0] = np.eye(128, dtype=f)
    cst[:, 1] = np.triu(np.ones((128, 128), f))
    cst[:, 2] = np.where(np.triu(np.ones((128, 128))) > 0, 0.0, -30000.0)
    cst[:, 3] = np.arange(1, 129, dtype=f)[None, :]
    sh["cst"] = cst
    return sh


_NC_CACHE = {}


def run(inp, nl=DEPTH, trace=False):
    inp = {k: np.asarray(v) for k, v in inp.items()}
    if nl not in _NC_CACHE:
        _NC_CACHE[nl] = B(nl).build()
    nc = _NC_CACHE[nl]
    sh = prep_shared(inp)
    in_maps = []
    for b in range(8):
        m = dict(sh)
        m["x"] = np.ascontiguousarray(inp["x"][b], dtype=np.float32)
        m["ccol"] = _col(np.asarray(inp["c"][b], dtype=np.float32), 8)
        in_maps.append(m)
    res = run_bass_kernel_spmd(nc, in_maps, core_ids=list(range(8)))
    return np.stack([np.asarray(r["y"]) for r in res.results], axis=0).astype(np.float32)


def kernel(**inputs):
    return run(inputs, DEPTH)
```

```python
import numpy as np
from contextlib import ExitStack
import concourse.bass as bass
import concourse.mybir as mybir
from concourse.bass_utils import run_bass_kernel_spmd

F32 = mybir.dt.float32
BF16 = mybir.dt.bfloat16
AF = mybir.ActivationFunctionType
ALU = mybir.AluOpType
AX = mybir.AxisListType

D = 1024
SEQ = 2048
NT = SEQ // 128
DEPTH = 4
INC = 3088
EPS = 1e-6
MAGIC = 12582912.0
TWO_PI = float(2 * np.pi)
RW1 = 16 + 16 + 16 + 512 + 512 + 36
RW2 = 32 + 2048 + 2048


class Reg:
    __slots__ = ("name", "w", "rs")

    def __init__(self, name=""):
        self.name = name
        self.w = None
        self.rs = {}


class Tok:
    __slots__ = ("sem", "val", "eng", "key")

    def __init__(self, sem, val, eng, key):
        self.sem, self.val, self.eng, self.key = sem, val, eng, key


class Stream:
    def __init__(self, name, eng):
        self.name, self.eng = name, eng
        self.sem = None
        self.key = None
        self.count = 0
        self.known = {}


class TK:
    def __init__(self, nc, es):
        self.nc, self.es = nc, es
        self.nsem = 0
        self.S = {}
        for n, e in (("pe", nc.tensor), ("act", nc.scalar), ("dve", nc.vector), ("pool", nc.gpsimd), ("sp", nc.sync)):
            s = Stream(n, e)
            self.S[n] = s
            self._rot(s)
        self.dsems = {}
        self.dpos = {}
        for q in ("sp", "pool", "act"):
            self.dsems[q] = [[self._new_sem("d" + q), 0, self._newkey()] for _ in range(6)]
            self.dpos[q] = 0
        self.ninst = 0

    def _newkey(self):
        self.nsem += 1
        return self.nsem

    def _new_sem(self, name):
        return self.es.enter_context(self.nc.semaphore(f"{name}_{self.nsem}"))

    def _rot(self, s):
        s.key = self._newkey()
        s.sem = self._new_sem("s" + s.name)
        s.count = 0

    def _wait(self, s, tok):
        if s.known.get(tok.key, 0) >= tok.val:
            return
        s.eng.wait_ge(tok.sem, tok.val)
        s.known[tok.key] = tok.val

    def _deps(self, s, R, W):
        for r in R:
            if r.w is not None:
                t = r.w
                if not (t.eng == s.name and s.name == "pe"):
                    self._wait(s, t)
        for w in W:
            if w.w is not None:
                t = w.w
                if not (t.eng == s.name and s.name == "pe"):
                    self._wait(s, t)
            for t in w.rs.values():
                if t.eng == s.name and s.name == "pe":
                    continue
                self._wait(s, t)

    def _mark(self, tok, R, W, rkey):
        for r in R:
            r.rs[rkey] = tok
        for w in W:
            w.w = tok
            w.rs = {}

    def op(self, en, fn, R, W):
        s = self.S[en]
        R = [getattr(r, "reg", r) for r in R]
        W = [getattr(w, "reg", w) for w in W]
        self._deps(s, R, W)
        ins = fn(s.eng)
        s.count += 1
        ins.then_inc(s.sem, 1)
        tok = Tok(s.sem, s.count, en, s.key)
        s.known[s.key] = s.count if en == "pe" else s.known.get(s.key, 0)
        self._mark(tok, R, W, en)
        self.ninst += 1
        if s.count >= 30000:
            self._rot(s)
        return tok

    def dma(self, q, out, in_, R, W):
        s = self.S[q]
        R = [getattr(r, "reg", r) for r in R]
        W = [getattr(w, "reg", w) for w in W]
        self._deps(s, R, W)
        lst = self.dsems[q]
        i = self.dpos[q]
        self.dpos[q] = (i + 1) % len(lst)
        ent = lst[i]
        if ent[1] > 0:
            self._wait(s, Tok(ent[0], ent[1], "dma", ent[2]))
        if ent[1] >= 30000:
            ent[0] = self._new_sem("d" + q)
            ent[1] = 0
            ent[2] = self._newkey()
        s.eng.dma_start(out=out, in_=in_).then_inc(ent[0], 16)
        ent[1] += 16
        tok = Tok(ent[0], ent[1], "dma", ent[2])
        self._mark(tok, R, W, ("d", ent[2]))
        self.ninst += 1
        return tok

    def barrier(self):
        toks = []
        for s in self.S.values():
            if s.count > 0:
                toks.append(Tok(s.sem, s.count, s.name, s.key))
        for q, lst in self.dsems.items():
            for ent in lst:
                if ent[1] > 0:
                    toks.append(Tok(ent[0], ent[1], "dma", ent[2]))
        for s in self.S.values():
            for t in toks:
                if t.eng == s.name:
                    continue
                self._wait(s, t)


class T:
    def __init__(self, h, name):
        self.h = h
        self.reg = Reg(name)

    def __getitem__(self, k):
        return self.h[k]


class B:
    def __init__(self, nlayers):
        self.nl = nlayers
        self.nc = bass.Bass("TRN2", target_bir_lowering=False)
        self.es = ExitStack()
        self.dr = {}

    def din(self, name, shape, dt=F32):
        self.dr[name] = self.nc.dram_tensor(name, list(shape), dt, kind="ExternalInput").ap()
        return self.dr[name]

    def sb(self, st, name, shape, dt=F32):
        self.uid = getattr(self, "uid", 0) + 1
        name = f"{name}_{self.uid}"
        return T(st.enter_context(self.nc.sbuf_tensor(name, list(shape), dt)), name)

    def tt(self, en, out, in0, in1, op, R, W):
        return self.tk.op(en, lambda e: e.tensor_tensor(out=out, in0=in0, in1=in1, op=op), R, W)

    def ts(self, en, out, in0, s1, s2, op0, op1, R, W):
        if op1 is None:
            return self.tk.op(en, lambda e: e.tensor_scalar(out=out, in0=in0, scalar1=s1, scalar2=None, op0=op0), R, W)
        return self.tk.op(en, lambda e: e.tensor_scalar(out=out, in0=in0, scalar1=s1, scalar2=s2, op0=op0, op1=op1), R, W)

    def stt(self, out, in0, sc, in1, op0, op1, R, W):
        return self.tk.op("dve", lambda e: e.scalar_tensor_tensor(out=out, in0=in0, scalar=sc, in1=in1, op0=op0, op1=op1), R, W)

    def cp(self, en, out, in_, R, W):
        if en == "act":
            return self.tk.op(en, lambda e: e.copy(out=out, in_=in_), R, W)
        return self.tk.op(en, lambda e: e.tensor_copy(out=out, in_=in_), R, W)

    def act(self, out, in_, func, R, W, bias=None, scale=None, accum=None):
        kw = {}
        if bias is not None:
            kw["bias"] = bias
        if scale is not None:
            kw["scale"] = scale
        if accum is not None:
            kw["accum_out"] = accum
        return self.tk.op("act", lambda e: e.activation(out=out, in_=in_, func=func, **kw), R, W)

    def mm(self, out, pairs, R, W):
        n = len(pairs)

        def fn(e):
            ins = None
            for i, (l, r) in enumerate(pairs):
                ins = e.matmul(out, lhsT=l, rhs=r, start=(i == 0), stop=(i == n - 1))
            return ins
        return self.tk.op("pe", fn, R, W)

    def mms(self, items, R, W):
        def fn(e):
            ins = None
            for (o, l, r) in items:
                ins = e.matmul(o, lhsT=l, rhs=r, start=True, stop=True)
            return ins
        return self.tk.op("pe", fn, R, W)

    def trs(self, items, ident, R, W):
        def fn(e):
            ins = None
            for (o, i) in items:
                ins = e.transpose(o, i, ident)
            return ins
        return self.tk.op("pe", fn, R, W)

    def pb(self):
        i = self.pbi
        self.pbi = (i + 1) % 7
        return i

    def rstd(self, ss, tmp, out, n, R, W):
        self.ts("dve", tmp, ss, 1.0 / n, EPS, ALU.mult, ALU.add, R, W)
        self.act(tmp, tmp, AF.Sqrt, W, W)
        self.tk.op("dve", lambda e: e.reciprocal(out=out, in_=tmp), W, W)

    def rangered(self, en, r, x, tmp, R):
        self.ts(en, tmp[0], x, 1.0 / TWO_PI, MAGIC, ALU.mult, ALU.add, R, [tmp[1]])
        self.ts(en, tmp[0], tmp[0], MAGIC, -TWO_PI, ALU.subtract, ALU.mult, [tmp[1]], [tmp[1]])
        self.tt(en, r[0], x, tmp[0], ALU.add, R + [tmp[1]], [r[1]])
        self.ts(en, r[0], r[0], 3.141592, -3.141592, ALU.min, ALU.max, [r[1]], [r[1]])

    def build(self):
        nc, es = self.nc, self.es
        din = self.din
        x_in = din("x", [SEQ, D])
        ccol = din("ccol", [128, 8])
        w_ada = din("w_ada", [DEPTH, D, 6 * D])
        badac = din("badac", [128, DEPTH, 48])
        n1c = din("n1c", [128, DEPTH, 8])
        n2c = din("n2c", [128, DEPTH, 8])
        w_in = din("w_in", [DEPTH, D, INC])
        w_out = din("w_out", [DEPTH, 1536, D])
        w_glu = din("w_glu", [DEPTH, 512, 512])
        convw = din("convw", [128, DEPTH, 12, 4])
        convb = din("convb", [128, DEPTH, 12])
        ynormc = din("ynormc", [128, DEPTH, 12])
        rows1 = din("rows1", [DEPTH, RW1])
        rows2 = din("rows2", [DEPTH, RW2])
        fnw = din("fnw", [1, D])
        lamc = din("lamc", [128, DEPTH, 3, 16])
        bblk = din("bblk", [DEPTH, 2, 128, 16, 128])
        cblk = din("cblk", [DEPTH, 2, 128, 16, 32])
        wr = din("wr", [DEPTH, D, 36])
        w_eg = din("w_eg", [DEPTH, 32, D, 512])
        w_eu = din("w_eu", [DEPTH, 32, D, 512])
        w_ed = din("w_ed", [DEPTH, 32, 512, D])
        cst = din("cst", [128, 4, 128])
        y_out = nc.dram_tensor("y", [SEQ, D], F32, kind="ExternalOutput").ap()
        xd = nc.dram_tensor("xd", [SEQ, D], F32, kind="Internal").ap()
        self.XD = [Reg(f"xd{t}") for t in range(NT)]
        self.YD = [Reg(f"yd{t}") for t in range(NT)]

        self.tk = tk = TK(nc, es)
        self.pbi = 0
        sb = self.sb
        self.ps = ps = es.enter_context(nc.psum_tensor("ps", [128, 8, 512], F32))
        self.PB = PB = [Reg(f"pb{i}") for i in range(8)]
        G = es
        cstt = sb(G, "cstt", [128, 4, 128])
        identb = sb(G, "identb", [128, 128], BF16)
        triub = sb(G, "triub", [128, 128], BF16)
        onesf = sb(G, "onesf", [128, 128])
        adac = sb(G, "adac", [128, DEPTH, 48])
        n1t = sb(G, "n1t", [128, DEPTH, 8])
        n2t = sb(G, "n2t", [128, DEPTH, 8])
        cact = sb(G, "cact", [128, 8])
        self.cact, self.w_ada, self.badac = cact, w_ada, badac
        tk.dma("sp", cstt[:], cst[:, :, :], [], [cstt])
        tk.dma("sp", n1t[:], n1c[:, :, :], [], [n1t])
        tk.dma("sp", n2t[:], n2c[:, :, :], [], [n2t])
        tk.dma("sp", cact[:], ccol[:, :], [], [cact])
        identf = cstt[:, 0, :]
        triuf = cstt[:, 1, :]
        maskb = cstt[:, 2, :]
        iotar = cstt[:, 3, :]
        self.cp("dve", identb[:], identf, [cstt], [identb])
        self.cp("dve", triub[:], triuf, [cstt], [triub])
        tk.op("dve", lambda e: e.memset(onesf[:], 1.0), [], [onesf])
        self.act(cact[:], cact[:], AF.Silu, [cact], [cact])
        self.identf, self.identb, self.triuf, self.triub = identf, identb, triuf, triub
        self.maskb, self.iotar, self.onesf, self.cstt = maskb, iotar, onesf, cstt
        self.adac, self.n1t, self.n2t = adac, n1t, n2t

        with ExitStack() as P0:
            wblk = [sb(P0, f"wblk{i}", [128, 8, 2048]) for i in range(2)]
            arow = sb(P0, "arow", [1, 6 * D])
            bad = sb(P0, "bad", [128, 48])
            tk.dma("sp", bad[:], badac[:, 0, :], [], [bad])
            n = 0
            for l in range(1):
                for j3 in range(3):
                    wb = wblk[n % 2]
                    n += 1
                    for kq in range(4):
                        src = w_ada[l, kq * 256:(kq + 1) * 256, j3 * 2048:(j3 + 1) * 2048].rearrange("(k p) n -> p k n", p=128)
                        tk.dma("sp" if kq % 2 else "act", wb[:, kq * 2:(kq + 1) * 2, :], src, [], [wb])
                    for jb in range(4):
                        bank = self.pb()
                        self.mm(ps[0:1, bank, :], [(cact[:, k:k + 1], wb[:, k, jb * 512:(jb + 1) * 512]) for k in range(8)], [wb, cact], [PB[bank]])
                        c0 = j3 * 2048 + jb * 512
                        self.cp("act", arow[0:1, c0:c0 + 512], ps[0:1, bank, :], [PB[bank]], [arow])
                bank = self.pb()
                self.mms([(ps[:, bank, j:j + 1], arow[0:1, j * 128:(j + 1) * 128], onesf[0:1, 0:1]) for j in range(48)], [arow, onesf], [PB[bank]])
                self.tt("dve", adac[:, l, :], ps[:, bank, 0:48], bad[:], ALU.add, [PB[bank], bad], [adac])
            tk.barrier()

        src_x = x_in
        for l in range(self.nl):
            self.mixer_phase(l, src_x, xd)
            src_x = xd
            self.moe_phase(l, xd, y_out if l == self.nl - 1 else xd, fnw, l == self.nl - 1)
        s = tk.S["sp"]
        for r in self.YD:
            if r.w is not None:
                tk._wait(s, r.w)
        tk.barrier()
        es.close()
        return nc

    def bcast_cols(self, col8, dst, R):
        tk, ps, PB = self.tk, self.ps, self.PB
        for h in range(2):
            bank = self.pb()
            for kk in range(4):
                k = h * 4 + kk
                self.ts("dve", self.dall[:, kk, :], self.identf, col8[:, k:k + 1], None, ALU.mult, None, R + [self.cstt], [self.dall])
                self.mm(ps[:, bank, kk * 128:(kk + 1) * 128], [(self.onesf[:], self.dall[:, kk, :])], [self.onesf, self.dall], [PB[bank]])
            self.cp("act", dst[:, h * 512:(h + 1) * 512], ps[:, bank, :], [PB[bank]], [dst])

    def mixer_phase(self, l, xsrc, xdst):
        nc, tk, ps, PB = self.nc, self.tk, self.ps, self.PB
        sb = self.sb
        dr = self.dr
        adac = self.adac
        with ExitStack() as M:
            Win = sb(M, "Win", [128, 8, INC], BF16)
            Wout = sb(M, "Wout", [128, 12, D], BF16)
            Wglu = sb(M, "Wglu", [128, 4, 512], BF16)
            Qc = sb(M, "Qc", [128, 16, 128])
            Qs = sb(M, "Qs", [128, 16, 128])
            Bb = sb(M, "Bb", [128, 16, 2, 128], BF16)
            Cb = sb(M, "Cb", [128, 16, 2, 32], BF16)
            mcol = sb(M, "mcol", [128, 16])
            r1 = sb(M, "r1", [128, RW1])
            A1c = sb(M, "A1c", [128, 8])
            abc = sb(M, "abc", [128, 16])
            cw = sb(M, "cw", [128, 12, 4])
            cbt = sb(M, "cbt", [128, 12])
            ync = sb(M, "ync", [128, 12])
            for k in range(8):
                tk.dma("pool", Win[:, k, :], dr["w_in"][l, k * 128:(k + 1) * 128, :], [], [Win])
            for k in range(12):
                tk.dma("pool", Wout[:, k, :], dr["w_out"][l, k * 128:(k + 1) * 128, :], [], [Wout])
            tk.dma("pool", Wglu[:], dr["w_glu"][l].rearrange("(k p) n -> p k n", p=128), [], [Wglu])
            tk.dma("sp", r1[:], dr["rows1"][l:l + 1, :].partition_broadcast(128), [], [r1])
            tk.dma("sp", cw[:], dr["convw"][:, l, :, :], [], [cw])
            tk.dma("sp", cbt[:], dr["convb"][:, l, :], [], [cbt])
            tk.dma("sp", ync[:], dr["ynormc"][:, l, :], [], [ync])
            dtb_bc = r1[:, 0:16]
            alog_bc = r1[:, 16:32]
            dssd_bc = r1[:, 32:48]
            bglu_bc = r1[:, 48:560]
            s5d_bc = r1[:, 560:1072]
            self.act(abc[:], alog_bc, AF.Exp, [r1], [abc])
            self.ts("dve", abc[:], abc[:], -1.0, None, ALU.mult, None, [abc], [abc])
            self.ts("dve", A1c[:], adac[:, l, 8:16], 1.0, None, ALU.add, None, [adac], [A1c])
            self.tt("dve", A1c[:], A1c[:], self.n1t[:, l, :], ALU.mult, [A1c, self.n1t], [A1c])
            shc = adac[:, l, 0:8]
            with ExitStack() as PP:
                r2 = sb(PP, "r2", [128, RW2])
                Tm = [sb(PP, f"Tm{i}", [128, 2048]) for i in range(6)]
                B1 = sb(PP, "B1", [128, 16, 128])
                B2 = sb(PP, "B2", [128, 16, 128])
                lct = sb(PP, "lct", [128, 3, 16])
                sc = [sb(PP, f"sc{i}", [128, 16]) for i in range(3)]
                C1 = sb(PP, "C1", [128, 16, 32])
                C2 = sb(PP, "C2", [128, 16, 32])
                self.dall = sb(PP, "dall", [128, 4, 128])
                gbc = sb(PP, "gbc", [128, D])
                tk.dma("sp", r2[:], dr["rows2"][l:l + 1, :].partition_broadcast(128), [], [r2])
                tk.dma("sp", B1[:], dr["bblk"][l, 0], [], [B1])
                tk.dma("act", B2[:], dr["bblk"][l, 1], [], [B2])
                tk.dma("sp", lct[:], dr["lamc"][:, l, :, :], [], [lct])
                tk.dma("sp", C1[:], dr["cblk"][l, 0], [], [C1])
                tk.dma("sp", C2[:], dr["cblk"][l, 1], [], [C2])
                ldt = r2[:, 0:32]
                lr = r2[:, 32:2080]
                li = r2[:, 2080:4128]
                T1, T2, T3, T4, T5, T6 = Tm
                v3 = lambda ap: ap.rearrange("p (g q) -> p g q", g=32)
                v16 = lambda ap: ap.rearrange("p (g q) -> p g q", g=16)
                stepR = sc[0]
                stR = T6[:, 0:32]
                self.act(stR, ldt, AF.Exp, [r2], [T6])
                stb = stR.unsqueeze(2).to_broadcast([128, 32, 64])
                self.tt("dve", v3(T1[:]), v3(lr), stb, ALU.mult, [r2, T6], [T1])
                self.tt("dve", v3(T2[:]), v3(li), stb, ALU.mult, [r2, T6], [T2])
                self.act(T3[:], T1[:], AF.Exp, [T1], [T3])
                self.rangered("dve", (T4[:], T4), T2[:], (T6[:], T6), [T2])
                self.act(T5[:], T4[:], AF.Sin, [T4], [T5])
                self.act(T6[:], T4[:], AF.Sin, [T4], [T6], scale=0.5)
                self.tt("dve", T6[:], T6[:], T6[:], ALU.mult, [T6], [T6])
                self.ts("dve", T6[:], T6[:], -2.0, 1.0, ALU.mult, ALU.add, [T6], [T6])
                self.tt("dve", T6[:], T3[:], T6[:], ALU.mult, [T3, T6], [T6])
                self.tt("dve", T5[:], T3[:], T5[:], ALU.mult, [T3, T5], [T5])
                self.tt("dve", T3[:], lr, lr, ALU.mult, [r2, T5, T6], [T3])
                self.tt("dve", T4[:], li, li, ALU.mult, [r2], [T4])
                self.tt("dve", T3[:], T3[:], T4[:], ALU.add, [T3, T4], [T3])
                tk.op("dve", lambda e: e.reciprocal(out=T3[:], in_=T3[:]), [T3], [T3])
                self.ts("dve", T6[:], T6[:], -1.0, None, ALU.add, None, [T6], [T6])
                self.tt("dve", T4[:], T6[:], lr, ALU.mult, [T6, r2], [T4])
                self.tt("dve", T1[:], T5[:], li, ALU.mult, [T5, r2], [T1])
                self.tt("dve", T4[:], T4[:], T1[:], ALU.add, [T4, T1], [T4])
                self.tt("dve", T4[:], T4[:], T3[:], ALU.mult, [T4, T3], [T4])
                self.tt("dve", T1[:], T5[:], lr, ALU.mult, [T5, r2, T4], [T1])
                self.tt("dve", T2[:], T6[:], li, ALU.mult, [T6, r2], [T2])
                self.tt("dve", T1[:], T1[:], T2[:], ALU.subtract, [T1, T2], [T1])
                self.tt("dve", T1[:], T1[:], T3[:], ALU.mult, [T1, T3], [T1])
                cre, cim = v16(T4[:]), v16(T1[:])
                self.tt("dve", v16(T2[:]), cre, B1[:], ALU.mult, [T4, B1], [T2])
                self.tt("dve", v16(T5[:]), cim, B2[:], ALU.mult, [T1, B2], [T5])
                self.tt("dve", Bb[:, :, 0, :], v16(T2[:]), v16(T5[:]), ALU.subtract, [T2, T5], [Bb])
                self.tt("dve", v16(T2[:]), cre, B2[:], ALU.mult, [T4, B2, Bb], [T2])
                self.tt("dve", v16(T5[:]), cim, B1[:], ALU.mult, [T1, B1, Bb], [T5])
                self.tt("dve", Bb[:, :, 1, :], v16(T2[:]), v16(T5[:]), ALU.add, [T2, T5], [Bb])
                stC, lrsC, lisC = sc
                self.act(stC[:], lct[:, 2, :], AF.Exp, [lct], [stC])
                self.tt("dve", lrsC[:], lct[:, 0, :], stC[:], ALU.mult, [lct, stC], [lrsC])
                self.tt("dve", lisC[:], lct[:, 1, :], stC[:], ALU.mult, [lct, stC], [lisC])
                self.act(mcol[:], lrsC[:], AF.Exp, [lrsC], [mcol])
                self.tt("dve", v16(T1[:]), lisC[:].unsqueeze(2).to_broadcast([128, 16, 128]),
                        self.iotar.unsqueeze(1).to_broadcast([128, 16, 128]), ALU.mult, [lisC, self.cstt], [T1])
                self.rangered("dve", (T2[:], T2), T1[:], (T3[:], T3), [T1])
                self.act(Qs[:].rearrange("p g q -> p (g q)"), T2[:], AF.Sin, [T2], [Qs])
                self.act(T3[:], T2[:], AF.Sin, [T2], [T3], scale=0.5)
                self.tt("dve", T3[:], T3[:], T3[:], ALU.mult, [T3], [T3])
                self.ts("dve", Qc[:].rearrange("p g q -> p (g q)"), T3[:], -2.0, 1.0, ALU.mult, ALU.add, [T3], [Qc])
                self.cp("dve", Cb[:, :, 0, :], C1[:], [C1], [Cb])
                self.ts("dve", Cb[:, :, 1, :], C2[:], -1.0, None, ALU.mult, None, [C2], [Cb])
                self.bcast_cols(adac[:, l, 16:24], gbc, [adac])
                for h in range(2):
                    self.tt("dve", Wout[:, :, h * 512:(h + 1) * 512], Wout[:, :, h * 512:(h + 1) * 512],
                            gbc[:, h * 512:(h + 1) * 512].unsqueeze(1).to_broadcast([128, 12, 512]), ALU.mult, [Wout, gbc], [Wout])
                tk.barrier()
            with ExitStack() as WK:
                xt2 = [sb(WK, "xta", [128, D]), sb(WK, "xtb", [128, D])]
                xnb = sb(WK, "xnb", [128, D], BF16)
                hT = sb(WK, "hT", [128, 8, 128], BF16)
                zs = sb(WK, "zs", [128, D])
                cin = sb(WK, "cin", [128, 12, 131])
                caccs = [sb(WK, f"cacc{i}", [128, 128]) for i in range(4)]
                cout = sb(WK, "cout", [128, 8, 128])
                BT = sb(WK, "BT", [128, 2, 128], BF16)
                CT = sb(WK, "CT", [128, 2, 128], BF16)
                xs = sb(WK, "xs", [128, D])
                xdb = sb(WK, "xdb", [128, D], BF16)
                xddb = sb(WK, "xddb", [128, D], BF16)
                Btok = sb(WK, "Btok", [128, 256], BF16)
                sm = sb(WK, "sm", [128, 10, 16])
                st1 = sb(WK, "st1", [128, 8])
                stF = sb(WK, "stF", [128, 4])
                stB = sb(WK, "stB", [128, 4])
                seg2 = [sb(WK, "sega", [128, 4, 128]), sb(WK, "segb", [128, 4, 128])]
                CBs = sb(WK, "CBs", [128, 2, 128])
                MT = sb(WK, "MT", [128, 16, 128], BF16)
                H = sb(WK, "H", [128, D])
                Hb = sb(WK, "Hb", [128, D], BF16)
                yv = sb(WK, "yv", [128, D])
                tA = sb(WK, "tA", [128, D])
                ynb = sb(WK, "ynb", [128, D], BF16)
                ycT = sb(WK, "ycT", [128, 12, 128], BF16)
                uT = sb(WK, "uT", [128, 4, 128], BF16)
                du = sb(WK, "du", [128, 512])
                t1 = sb(WK, "t1", [128, 4, 128])
                t2 = sb(WK, "t2", [128, 4, 128])
                rin = sb(WK, "rin", [128, 4, 2, 128])
                rr = sb(WK, "rr", [128, 4, 2, 128])
                sT = sb(WK, "sT", [128, 4, 2, 128], BF16)
                carry = sb(WK, "carry", [128, 16, 2])
                y5 = sb(WK, "y5", [128, 512])
                q5 = sb(WK, "q5", [128, 512])
                yg = sb(WK, "yg", [128, 512])
                ygb = sb(WK, "ygb", [128, 512], BF16)
                ygT = sb(WK, "ygT", [128, 4, 128], BF16)
                tk.op("pool", lambda e: e.memset(cin[:], 0.0), [], [cin])
                tk.op("pool", lambda e: e.memset(H[:], 0.0), [], [H])
                tk.op("pool", lambda e: e.memset(Hb[:], 0.0), [], [Hb])
                tk.op("pool", lambda e: e.memset(carry[:], 0.0), [], [carry])
                dt_, ad_, acs_, ea_, ds_, dtds_, cd_, tm_ = [sm[:, i, :] for i in range(8)]
                psb = lambda b: ps[:, b, :].bitcast(BF16)

                t3 = sb(WK, "t3", [128, 4, 128])
                t4 = sb(WK, "t4", [128, 4, 128])
                v16h = lambda ap: ap.rearrange("p (h q) -> p h q", h=16)
                ACTI = AF.Identity

                def front_steps(t):
                    S = []
                    xt = xt2[t % 2]
                    r0 = t * 128

                    def f1():
                        tk.dma("sp", xt[:], xsrc[r0:r0 + 128, :], [self.XD[t]], [xt])
                        self.act(xnb[:], xt[:], AF.Square, [xt], [xnb, stF], accum=stF[:, 0:1])
                        self.rstd(stF[:, 0:1], stF[:, 1:2], stF[:, 2:3], D, [stF], [stF])
                        self.act(xnb[:], xt[:], AF.Copy, [xt, stF], [xnb], scale=stF[:, 2:3])
                        bk = self.pb()
                        pv = psb(bk).rearrange("p (k q) -> p k q", k=8)
                        self.trs([(pv[:, k, :], xnb[:, k * 128:(k + 1) * 128]) for k in range(8)], self.identb[:], [xnb, self.identb], [PB[bk]])
                        for k in range(8):
                            self.act(hT[:, k, :], pv[:, k, :], ACTI, [PB[bk], A1c, adac], [hT], scale=A1c[:, k:k + 1], bias=shc[:, k:k + 1])
                    S.append(f1)

                    def f2(h):
                        bk = self.pb()
                        self.mm(ps[:, bk, :], [(hT[:, k, :], Win[:, k, h * 512:(h + 1) * 512]) for k in range(8)], [hT, Win], [PB[bk]])
                        self.act(zs[:, h * 512:(h + 1) * 512], ps[:, bk, :], AF.Silu, [PB[bk]], [zs])
                    S.append(lambda: f2(0))
                    S.append(lambda: f2(1))

                    def f3():
                        bk = self.pb()
                        self.mm(ps[:, bk, 0:16], [(hT[:, k, :], Win[:, k, 2560:2576]) for k in range(8)], [hT, Win], [PB[bk]])
                        self.tt("dve", tm_, ps[:, bk, 0:16], dtb_bc, ALU.add, [PB[bk], r1], [sm])
                        self.act(tm_, tm_, AF.Exp, [sm], [sm])
                        self.act(dt_, tm_, AF.Ln, [sm], [sm], bias=1.0)
                        self.tt("dve", ad_, dt_, abc[:], ALU.mult, [sm, abc], [sm])
                        bk = self.pb()
                        self.mm(ps[:, bk, :], [(hT[:, k, :], Win[:, k, 2576:3088]) for k in range(8)], [hT, Win], [PB[bk]])
                        self.tt("dve", du[:], ps[:, bk, :], s5d_bc, ALU.mult, [PB[bk], r1], [du])
                    S.append(f3)

                    def f4(g3):
                        bk = self.pb()
                        for cc in range(4):
                            c = g3 * 4 + cc
                            self.mm(ps[:, bk, cc * 128:(cc + 1) * 128],
                                    [(Win[:, k, 1024 + c * 128:1024 + (c + 1) * 128], hT[:, k, :]) for k in range(8)], [hT, Win], [PB[bk]])
                        self.cp("act", cin[:, g3 * 4:(g3 + 1) * 4, 3:131], ps[:, bk, :].rearrange("p (c q) -> p c q", c=4), [PB[bk]], [cin])
                    for g3 in range(3):
                        S.append(lambda g3=g3: f4(g3))

                    def f5():
                        bk = self.pb()
                        for c in range(4):
                            self.mm(ps[:, bk, c * 128:(c + 1) * 128],
                                    [(Win[:, k, 2576 + c * 128:2576 + (c + 1) * 128], hT[:, k, :]) for k in range(8)], [hT, Win], [PB[bk]])
                        self.cp("act", uT[:], ps[:, bk, :].rearrange("p (c q) -> p c q", c=4), [PB[bk]], [uT])
                    S.append(f5)

                    def f6(c):
                        cacc = caccs[c % 4]
                        ca = cacc[:]
                        self.act(ca, cin[:, c, 0:128], ACTI, [cin, cw, cbt], [cacc], scale=cw[:, c, 0:1], bias=cbt[:, c:c + 1])
                        for k in range(1, 4):
                            self.stt(ca, cin[:, c, k:k + 128], cw[:, c, k:k + 1], ca, ALU.mult, ALU.add, [cin, cw, cacc], [cacc])
                        if c < 8:
                            self.act(cout[:, c, :], ca, AF.Silu, [cacc], [cout])
                        elif c < 10:
                            self.act(BT[:, c - 8, :], ca, AF.Silu, [cacc], [BT])
                        else:
                            self.act(CT[:, c - 10, :], ca, AF.Silu, [cacc], [CT])
                    for c in range(12):
                        S.append(lambda c=c: f6(c))

                    def f7():
                        self.cp("pool", cin[:, :, 0:3], cin[:, :, 128:131], [cin], [cin])
                        for h in range(2):
                            bk = self.pb()
                            self.trs([(ps[:, bk, cc * 128:(cc + 1) * 128], cout[:, h * 4 + cc, :]) for cc in range(4)], self.identf, [cout, self.cstt], [PB[bk]])
                            self.cp("act", xs[:, h * 512:(h + 1) * 512], ps[:, bk, :], [PB[bk]], [xs])
                        bk = self.pb()
                        pvb = psb(bk)
                        self.trs([(pvb[:, g * 128:(g + 1) * 128], BT[:, g, :]) for g in range(2)], self.identb[:], [BT, self.identb], [PB[bk]])
                        self.cp("dve", Btok[:], pvb[:, 0:256], [PB[bk]], [Btok])
                    S.append(f7)
                    return S

                def ssd_steps(t):
                    S = []

                    def s1():
                        bk = self.pb()
                        self.mm(ps[:, bk, 0:16], [(self.triuf, ad_)], [self.cstt, sm], [PB[bk]])
                        self.cp("dve", acs_, ps[:, bk, 0:16], [PB[bk]], [sm])
                        self.act(ea_, acs_, AF.Exp, [sm], [sm])
                        bkc = self.pb()
                        self.mms([(ps[:, bkc, g * 128:(g + 1) * 128], BT[:, g, :], CT[:, g, :]) for g in range(2)], [BT, CT], [PB[bkc]])
                        self.cp("act", CBs[:], ps[:, bkc, 0:256].rearrange("p (g q) -> p g q", g=2), [PB[bkc]], [CBs])
                    S.append(s1)

                    def sq(q):
                        seg = seg2[q % 2]
                        h0 = q * 4
                        g = h0 // 8
                        bk = self.pb()
                        self.mms([(ps[:, bk, hh * 128:(hh + 1) * 128], ad_[:, h0 + hh:h0 + hh + 1].to_broadcast([128, 128]), self.triuf) for hh in range(4)],
                                 [sm, self.cstt], [PB[bk]])
                        pv4 = ps[:, bk, :].rearrange("p (h q) -> p h q", h=4)
                        self.act(cd_[:, h0:h0 + 4], pv4[:, :, 127], AF.Exp, [PB[bk]], [sm])
                        self.tt("dve", tm_[:, h0:h0 + 4], pv4[:, :, 127], acs_[:, h0:h0 + 4], ALU.subtract, [PB[bk], sm], [sm])
                        for hh in range(4):
                            self.stt(seg[:, hh, :], pv4[:, hh, :], acs_[:, h0 + hh:h0 + hh + 1], self.maskb, ALU.subtract, ALU.add, [PB[bk], sm, self.cstt], [seg])
                        self.act(seg[:], seg[:], AF.Exp, [seg], [seg])
                        self.tt("dve", MT[:, h0:h0 + 4, :], seg[:], CBs[:, g:g + 1, :].to_broadcast([128, 4, 128]), ALU.mult, [seg, CBs], [MT])
                    for q in range(4):
                        S.append(lambda q=q: sq(q))

                    def s7():
                        self.act(ds_, tm_, AF.Exp, [sm], [sm])
                        self.tt("dve", dtds_, dt_, ds_, ALU.mult, [sm], [sm])
                        self.tt("dve", v16h(xdb[:]), v16h(xs[:]), dt_.unsqueeze(2).to_broadcast([128, 16, 64]), ALU.mult, [xs, sm], [xdb])
                        self.tt("dve", v16h(xddb[:]), v16h(xs[:]), dtds_.unsqueeze(2).to_broadcast([128, 16, 64]), ALU.mult, [xs, sm], [xddb])
                    S.append(s7)

                    def sy(g):
                        bko = self.pb()
                        self.mm(ps[:, bko, :], [(CT[:, g, :], Hb[:, g * 512:(g + 1) * 512])], [CT, Hb], [PB[bko]])
                        self.tt("dve", v16h(tA[:])[:, g * 8:(g + 1) * 8, :], ps[:, bko, :].rearrange("p (h q) -> p h q", h=8),
                                ea_[:, g * 8:(g + 1) * 8].unsqueeze(2).to_broadcast([128, 8, 64]), ALU.mult, [PB[bko], sm], [tA])
                        bkd = self.pb()
                        self.mms([(ps[:, bkd, hh * 64:(hh + 1) * 64], MT[:, g * 8 + hh, :], xdb[:, (g * 8 + hh) * 64:(g * 8 + hh + 1) * 64]) for hh in range(8)],
                                 [MT, xdb], [PB[bkd]])
                        self.tt("dve", yv[:, g * 512:(g + 1) * 512], ps[:, bkd, :], tA[:, g * 512:(g + 1) * 512], ALU.add, [PB[bkd], tA], [yv])
                    S.append(lambda: sy(0))
                    S.append(lambda: sy(1))

                    def s10():
                        self.tt("dve", v16h(H[:]), v16h(H[:]), cd_.unsqueeze(2).to_broadcast([128, 16, 64]), ALU.mult, [H, sm], [H])
                        for g in range(2):
                            bk = self.pb()
                            self.mm(ps[:, bk, :], [(Btok[:, g * 128:(g + 1) * 128], xddb[:, g * 512:(g + 1) * 512])], [Btok, xddb], [PB[bk]])
                            self.tt("dve", H[:, g * 512:(g + 1) * 512], ps[:, bk, :], H[:, g * 512:(g + 1) * 512], ALU.add, [PB[bk], H], [H])
                        self.cp("act", Hb[:], H[:], [H], [Hb])
                    S.append(s10)

                    def s11():
                        self.tt("dve", v16h(tA[:]), v16h(xs[:]), dssd_bc.unsqueeze(2).to_broadcast([128, 16, 64]), ALU.mult, [xs, r1], [tA])
                        self.tt("dve", yv[:], yv[:], tA[:], ALU.add, [yv, tA], [yv])
                        self.tt("dve", yv[:], yv[:], zs[:], ALU.mult, [yv, zs], [yv])
                    S.append(s11)

                    def s12():
                        for g in range(2):
                            self.act(tA[:, g * 512:(g + 1) * 512], yv[:, g * 512:(g + 1) * 512], AF.Square, [yv], [tA, st1], accum=st1[:, 3 + g:4 + g])
                        self.rstd(st1[:, 3:5], st1[:, 5:7], st1[:, 3:5], 512, [st1], [st1])
                        for g in range(2):
                            self.act(ynb[:, g * 512:(g + 1) * 512], yv[:, g * 512:(g + 1) * 512], AF.Copy, [yv, st1], [ynb], scale=st1[:, 3 + g:4 + g])
                        bk = self.pb()
                        pv = psb(bk).rearrange("p (k q) -> p k q", k=8)
                        self.trs([(pv[:, k, :], ynb[:, k * 128:(k + 1) * 128]) for k in range(8)], self.identb[:], [ynb, self.identb], [PB[bk]])
                        self.tt("dve", ycT[:, 0:8, :], pv, ync[:, 0:8].unsqueeze(2).to_broadcast([128, 8, 128]), ALU.mult, [PB[bk], ync], [ycT])
                    S.append(s12)
                    return S

                def s5_steps(t, bky):
                    S = []

                    bub = {}

                    def bu(c):
                        gp0 = c * 4
                        for hb in range(2):
                            bk = self.pb()
                            bub[(c, hb)] = bk
                            self.mms([(ps[:, bk, (gg * 2 + ri) * 128:(gg * 2 + ri + 1) * 128], Bb[:, gp0 + hb * 2 + gg, ri, :], uT[:, c, :])
                                      for gg in range(2) for ri in range(2)], [Bb, uT], [PB[bk]])

                    def pre(c):
                        gp0 = c * 4
                        qc = Qc[:, gp0:gp0 + 4, :]
                        qs = Qs[:, gp0:gp0 + 4, :]
                        for hb in range(2):
                            bk = bub[(c, hb)]
                            pvv = ps[:, bk, :].rearrange("p (g r q) -> p g r q", g=2, r=2)
                            bre, bim = pvv[:, :, 0, :], pvv[:, :, 1, :]
                            qch = Qc[:, gp0 + hb * 2:gp0 + hb * 2 + 2, :]
                            qsh = Qs[:, gp0 + hb * 2:gp0 + hb * 2 + 2, :]
                            hs = slice(hb * 2, hb * 2 + 2)
                            self.tt("dve", t1[:, hs, :], qch, bre, ALU.mult, [Qc, PB[bk]], [t1])
                            self.tt("dve", t2[:, hs, :], qsh, bim, ALU.mult, [Qs, PB[bk]], [t2])
                            self.tt("dve", t3[:, hs, :], qch, bim, ALU.mult, [Qc, PB[bk]], [t3])
                            self.tt("dve", t4[:, hs, :], qsh, bre, ALU.mult, [Qs, PB[bk]], [t4])
                        self.tt("dve", rin[:, :, 0, :], t1[:], t2[:], ALU.add, [t1, t2], [rin])
                        self.tt("dve", rin[:, :, 1, :], t3[:], t4[:], ALU.subtract, [t3, t4], [rin])

                    def scan(c):
                        gp0 = c * 4
                        for gg in range(4):
                            for ri in range(2):
                                gp = gp0 + gg
                                tk.op("dve", lambda e, gg=gg, ri=ri, gp=gp: e.tensor_tensor_scan(
                                    out=rr[:, gg, ri, :], data0=mcol[:, gp:gp + 1].to_broadcast([128, 128]), data1=rin[:, gg, ri, :],
                                    initial=carry[:, gp, ri:ri + 1], op0=ALU.mult, op1=ALU.add), [mcol, rin, carry], [rr])

                    def post(c):
                        gp0 = c * 4
                        qc = Qc[:, gp0:gp0 + 4, :]
                        qs = Qs[:, gp0:gp0 + 4, :]
                        rre, rim = rr[:, :, 0, :], rr[:, :, 1, :]
                        self.tt("dve", t1[:], qc, rre, ALU.mult, [Qc, rr], [t1])
                        self.tt("dve", t2[:], qs, rim, ALU.mult, [Qs, rr], [t2])
                        self.tt("dve", t3[:], qc, rim, ALU.mult, [Qc, rr], [t3])
                        self.tt("dve", t4[:], qs, rre, ALU.mult, [Qs, rr], [t4])
                        self.tt("dve", sT[:, :, 0, :], t1[:], t2[:], ALU.subtract, [t1, t2], [sT])
                        self.tt("dve", sT[:, :, 1, :], t3[:], t4[:], ALU.add, [t3, t4], [sT])
                        self.tt("dve", carry[:, gp0:gp0 + 4, 0], t1[:, :, 127], t2[:, :, 127], ALU.subtract, [t1, t2], [carry])
                        self.tt("dve", carry[:, gp0:gp0 + 4, 1], t3[:, :, 127], t4[:, :, 127], ALU.add, [t3, t4], [carry])
                        for gg in range(4):
                            gp = gp0 + gg
                            self.mm(ps[:, bky, gp * 32:(gp + 1) * 32], [(sT[:, gg, 0, :], Cb[:, gp, 0, :]), (sT[:, gg, 1, :], Cb[:, gp, 1, :])], [sT, Cb], [PB[bky]])
                    for c in range(4):
                        S.append(lambda c=c: bu(c))
                        S.append(lambda c=c: pre(c))
                        S.append(lambda c=c: scan(c))
                        S.append(lambda c=c: post(c))
                    return S

                def back_steps(t, bky):
                    S = []
                    xt = xt2[t % 2]
                    r0 = t * 128

                    def b1():
                        self.tt("dve", q5[:], y5[:], y5[:], ALU.mult, [y5], [q5])
                        self.ts("dve", q5[:], q5[:], 0.044715, 1.0, ALU.mult, ALU.add, [q5], [q5])
                        self.tt("dve", q5[:], q5[:], y5[:], ALU.mult, [q5, y5], [q5])
                        self.act(q5[:], q5[:], AF.Sigmoid, [q5], [q5], scale=1.5957691216057308)
                    S.append(b1)

                    def b2():
                        self.tt("dve", yg[:], y5[:], q5[:], ALU.mult, [y5, q5], [yg])
                        self.cp("act", ygb[:], yg[:], [yg], [ygb])
                        bk = self.pb()
                        pvb = psb(bk)
                        self.trs([(pvb[:, k * 128:(k + 1) * 128], ygb[:, k * 128:(k + 1) * 128]) for k in range(4)], self.identb[:], [ygb, self.identb], [PB[bk]])
                        self.cp("act", ygT[:], pvb[:, 0:512].rearrange("p (k q) -> p k q", k=4), [PB[bk]], [ygT])
                    S.append(b2)

                    def b3():
                        bk = self.pb()
                        self.mm(ps[:, bk, :], [(ygT[:, k, :], Wglu[:, k, :]) for k in range(4)], [ygT, Wglu], [PB[bk]])
                        self.tt("dve", q5[:], ps[:, bk, :], bglu_bc, ALU.add, [PB[bk], r1], [q5])
                        self.act(q5[:], q5[:], AF.Sigmoid, [q5], [q5])
                    S.append(b3)

                    def b4():
                        self.tt("dve", yg[:], yg[:], q5[:], ALU.mult, [yg, q5], [yg])
                        self.act(q5[:], yg[:], AF.Square, [yg], [q5, stB], accum=stB[:, 0:1])
                        self.rstd(stB[:, 0:1], stB[:, 1:2], stB[:, 2:3], 512, [stB], [stB])
                        self.act(ygb[:], yg[:], AF.Copy, [yg, stB], [ygb], scale=stB[:, 2:3])
                    S.append(b4)

                    def b5():
                        bk = self.pb()
                        pvb = psb(bk)
                        self.trs([(pvb[:, k * 128:(k + 1) * 128], ygb[:, k * 128:(k + 1) * 128]) for k in range(4)], self.identb[:], [ygb, self.identb], [PB[bk]])
                        self.tt("dve", ycT[:, 8:12, :], pvb[:, 0:512].rearrange("p (k q) -> p k q", k=4),
                                ync[:, 8:12].unsqueeze(2).to_broadcast([128, 4, 128]), ALU.mult, [PB[bk], ync], [ycT])
                    S.append(b5)

                    def b6(h):
                        bk = self.pb()
                        self.mm(ps[:, bk, :], [(ycT[:, k, :], Wout[:, k, h * 512:(h + 1) * 512]) for k in range(12)], [ycT, Wout], [PB[bk]])
                        self.tt("dve", tA[:, h * 512:(h + 1) * 512], ps[:, bk, :], xt[:, h * 512:(h + 1) * 512], ALU.add, [PB[bk], xt], [tA])
                    S.append(lambda: b6(0))

                    def b7():
                        b6(1)
                        tk.dma("sp", xdst[r0:r0 + 128, :], tA[:], [tA], [self.XD[t]])
                    S.append(b7)
                    return S

                def interleave(A, Bs, ratio=1):
                    ia = ib = 0
                    while ia < len(A) or ib < len(Bs):
                        for _ in range(ratio):
                            if ib < len(Bs):
                                Bs[ib]()
                                ib += 1
                        if ia < len(A):
                            A[ia]()
                            ia += 1

                for f in front_steps(0):
                    f()
                for t in range(NT):
                    bky = 7
                    nf = front_steps(t + 1) if t + 1 < NT else []
                    A, Bs = ssd_steps(t), s5_steps(t, bky)
                    if nf:
                        Bs = Bs[:8] + [nf[0]] + Bs[8:]
                    interleave(A, Bs)
                    self.tt("dve", y5[:], ps[:, bky, :], du[:], ALU.add, [PB[bky], du], [y5])
                    interleave(back_steps(t, bky), nf[1:], ratio=3)
                tk.barrier()
            tk.barrier()

    def moe_phase(self, l, xsrc, xdst, fnw, last):
        nc, tk, ps, PB = self.nc, self.tk, self.ps, self.PB
        sb = self.sb
        dr = self.dr
        adac = self.adac
        OUTR = self.YD if last else self.XD
        with ExitStack() as E:
            xall = [sb(E, f"xall{t}", [128, D]) for t in range(NT)]
            h2T = [sb(E, f"h2T{m}", [128, 8, 512], BF16) for m in range(4)]
            Wg = [sb(E, f"Wg{i}", [128, 8, 512], BF16) for i in range(2)]
            Wu = [sb(E, f"Wu{i}", [128, 8, 512], BF16) for i in range(2)]
            Wd = [sb(E, f"Wd{i}", [128, 4, D], BF16) for i in range(2)]
            actb = [sb(E, f"actb{i}", [128, 4, 512], BF16) for i in range(2)]
            sg = [sb(E, f"sg{i}", [128, 512]) for i in range(2)]
            Gall = sb(E, "Gall", [128, NT, 32])
            gfbc = sb(E, "gfbc", [128, D])
            h2f = sb(E, "h2f", [128, 8, 128])
            xn = sb(E, "xn", [128, D])
            junk = sb(E, "junk", [128, D], BF16)
            Wr = sb(E, "Wr", [128, 8, 36])
            r1 = sb(E, "r1e", [128, RW1])
            A2c = sb(E, "A2c", [128, 8])
            st1 = sb(E, "st1e", [128, 8])
            LG = sb(E, "LG", [128, 36])
            rt = sb(E, "rt", [128, 6, 32])
            sm = sb(E, "sme", [128, 16])
            self.dall = sb(E, "dalle", [128, 4, 128])
            do_ada = (l + 1 < self.nl)
            if do_ada:
                wab = sb(E, "wab", [128, 8, 256])
                arw = sb(E, "arw", [1, 256])
                badl = sb(E, "badl", [128, 48])
                tk.dma("sp", badl[:], self.badac[:, l + 1, :], [], [badl])
            BK7 = PB[7]

            def ada_issue(i):
                src = self.w_ada[l + 1, :, i * 256:(i + 1) * 256].rearrange("(k p) n -> p k n", p=128)
                tk.dma("sp", wab[:], src, [], [wab])

            def ada_consume(i):
                bank = self.pb()
                self.mm(ps[0:1, bank, 0:256], [(self.cact[:, k:k + 1], wab[:, k, :]) for k in range(8)], [wab, self.cact], [PB[bank]])
                self.cp("act", arw[0:1, :], ps[0:1, bank, 0:256], [PB[bank]], [arw])
                self.mms([(ps[:, 7, 2 * i + jj:2 * i + jj + 1], arw[0:1, jj * 128:(jj + 1) * 128], self.onesf[0:1, 0:1]) for jj in range(2)],
                         [arw, self.onesf], [BK7])
            PBUF = [(xn, h2f, st1, LG, rt, sm),
                    (sb(E, "xn2", [128, D]), sb(E, "h2f2", [128, 8, 128]), sb(E, "st1e2", [128, 8]), sb(E, "LG2", [128, 36]),
                     sb(E, "rt2", [128, 6, 32]), sb(E, "sme2", [128, 16]))]
            tk.dma("sp", Wr[:], dr["wr"][l].rearrange("(k p) n -> p k n", p=128), [], [Wr])
            tk.dma("sp", r1[:], dr["rows1"][l:l + 1, :].partition_broadcast(128), [], [r1])
            brc = r1[:, 1072:1108]
            self.ts("dve", A2c[:], adac[:, l, 32:40], 1.0, None, ALU.add, None, [adac], [A2c])
            self.tt("dve", A2c[:], A2c[:], self.n2t[:, l, :], ALU.mult, [A2c, self.n2t], [A2c])
            shf = adac[:, l, 24:32]
            self.bcast_cols(adac[:, l, 40:48], gfbc, [adac])

            def load_expert(e):
                i = e % 2
                for k0 in range(0, 8, 4):
                    tk.dma("pool", Wg[i][:, k0:k0 + 4, :], dr["w_eg"][l, e, k0 * 128:(k0 + 4) * 128, :].rearrange("(k p) n -> p k n", p=128), [], [Wg[i]])
                    tk.dma("pool", Wu[i][:, k0:k0 + 4, :], dr["w_eu"][l, e, k0 * 128:(k0 + 4) * 128, :].rearrange("(k p) n -> p k n", p=128), [], [Wu[i]])
                for k0 in range(0, 4, 2):
                    tk.dma("pool", Wd[i][:, k0:k0 + 2, :], dr["w_ed"][l, e, k0 * 128:(k0 + 2) * 128, :].rearrange("(k p) n -> p k n", p=128), [], [Wd[i]])
                tk.op("pool", lambda en: en.tensor_tensor(out=Wd[i][:], in0=Wd[i][:], in1=gfbc[:].unsqueeze(1).to_broadcast([128, 4, D]), op=ALU.mult), [Wd[i], gfbc], [Wd[i]])

            GR = [Reg(f"g{t}") for t in range(NT)]

            def pro_steps(t):
                r0 = t * 128
                xa = xall[t]
                xn, h2f, st1, LG, rt, sm = PBUF[t % 2]

                def p1():
                    tk.dma("sp", xa[:], xsrc[r0:r0 + 128, :], [self.XD[t]], [xa])
                    self.act(junk[:], xa[:], AF.Square, [xa], [junk, st1], accum=st1[:, 0:1])
                    self.rstd(st1[:, 0:1], st1[:, 1:2], st1[:, 2:3], D, [st1], [st1])
                    self.ts("dve", xn[:], xa[:], st1[:, 2:3], None, ALU.mult, None, [xa, st1], [xn])

                def p2():
                    for h in range(2):
                        bk = self.pb()
                        self.trs([(ps[:, bk, cc * 128:(cc + 1) * 128], xn[:, (h * 4 + cc) * 128:(h * 4 + cc + 1) * 128]) for cc in range(4)], self.identf, [xn, self.cstt], [PB[bk]])
                        pv = ps[:, bk, :].rearrange("p (k q) -> p k q", k=4)
                        for cc in range(4):
                            k = h * 4 + cc
                            self.act(h2f[:, k, :], pv[:, cc, :], AF.Identity, [PB[bk], A2c, adac], [h2f], scale=A2c[:, k:k + 1], bias=shf[:, k:k + 1])
                    self.cp("dve", h2T[t // 4][:, :, (t % 4) * 128:(t % 4 + 1) * 128], h2f[:], [h2f], [h2T[t // 4]])

                def p3():
                    bk = self.pb()
                    self.mm(ps[:, bk, 0:36], [(h2f[:, k, :], Wr[:, k, :]) for k in range(8)], [h2f, Wr], [PB[bk]])
                    self.tt("dve", LG[:], ps[:, bk, 0:36], brc, ALU.add, [PB[bk], r1], [LG])
                    gl = LG[:, 0:4]
                    el = LG[:, 4:36]
                    gmax, ngmax, gsum, gw, m1, m2, d21, e2, den, g1, g2 = [sm[:, i:i + 1] for i in range(11)]
                    ohg, pen, eg = rt[:, 0, 0:4], rt[:, 0, 8:12], rt[:, 0, 16:20]
                    ME, oh1, ME2, oh2 = rt[:, 1, :], rt[:, 2, :], rt[:, 3, :], rt[:, 4, :]
                    V = "dve"
                    tk.op(V, lambda e: e.reduce_max(out=gmax, in_=gl, axis=AX.X), [LG], [sm])
                    self.ts(V, ohg, gl, gmax, None, ALU.is_equal, None, [LG, sm], [rt])
                    self.ts(V, ngmax, gmax, -1.0, None, ALU.mult, None, [sm], [sm])
                    self.act(eg, gl, AF.Exp, [LG, sm], [rt, sm], bias=ngmax, accum=gsum)
                    tk.op(V, lambda e: e.reciprocal(out=gw, in_=gsum), [sm], [sm])
                    self.ts(V, pen, ohg, -1.0, 1e30, ALU.add, ALU.mult, [rt], [rt])
                    self.tt(V, ME.rearrange("p (g q) -> p g q", g=4), el.rearrange("p (g q) -> p g q", g=4),
                            pen.unsqueeze(2).to_broadcast([128, 4, 8]), ALU.add, [LG, rt], [rt])
                    tk.op(V, lambda e: e.reduce_max(out=m1, in_=ME, axis=AX.X), [rt], [sm])
                    self.ts(V, oh1, ME, m1, None, ALU.is_equal, None, [rt, sm], [rt])
                    self.stt(ME2, oh1, -1e30, ME, ALU.mult, ALU.add, [rt], [rt])
                    tk.op(V, lambda e: e.reduce_max(out=m2, in_=ME2, axis=AX.X), [rt], [sm])
                    self.ts(V, oh2, ME2, m2, None, ALU.is_equal, None, [rt, sm], [rt])
                    self.tt(V, d21, m2, m1, ALU.subtract, [sm], [sm])
                    self.act(e2, d21, AF.Exp, [sm], [sm])
                    self.ts(V, den, e2, 1.0, None, ALU.add, None, [sm], [sm])
                    tk.op(V, lambda e: e.reciprocal(out=den, in_=den), [sm], [sm])
                    self.tt(V, g1, gw, den, ALU.mult, [sm], [sm])
                    self.tt(V, g2, g1, e2, ALU.mult, [sm], [sm])
                    self.ts(V, oh1, oh1, g1, None, ALU.mult, None, [rt, sm], [rt])
                    self.stt(Gall[:, t, :], oh2, g2, oh1, ALU.mult, ALU.add, [rt, sm], [GR[t]])
                return [p1, p2, p3]

            def epilogue(t):
                r0 = t * 128
                xa = xall[t]
                if last:
                    self.act(junk[:], xa[:], AF.Square, [xa], [junk, st1], accum=st1[:, 0:1])
                    self.rstd(st1[:, 0:1], st1[:, 1:2], st1[:, 2:3], D, [st1], [st1])
                    self.ts("dve", xn[:], xa[:], st1[:, 2:3], None, ALU.mult, None, [xa, st1], [xn])
                    self.tt("dve", xa[:], xn[:], gfbc[:], ALU.mult, [xn, gfbc], [xa])
                tk.dma("sp", xdst[r0:r0 + 128, :], xa[:], [xa], [OUTR[t]])

            load_expert(0)
            for e in range(32):
                i = e % 2
                if e + 1 < 32:
                    load_expert(e + 1)
                if last and e == 31:
                    tk.dma("sp", gfbc[:], fnw[0:1, :].partition_broadcast(128), [], [gfbc])
                if do_ada:
                    if 2 <= e <= 25:
                        ada_consume(e - 2)
                    if 1 <= e <= 24:
                        ada_issue(e - 1)
                    if e == 26:
                        self.tt("dve", adac[:, l + 1, :], ps[:, 7, 0:48], badl[:], ALU.add, [BK7, badl], [adac])
                for m in range(4):
                    PS = []
                    if e == 0:
                        if m == 0:
                            for pr in range(2):
                                sa, sb_ = pro_steps(2 * pr), pro_steps(2 * pr + 1)
                                for f in (sa[0], sb_[0], sa[1], sb_[1], sa[2], sb_[2]):
                                    f()
                        if m < 3:
                            t0_ = 4 * (m + 1)
                            for pr in range(2):
                                sa, sb_ = pro_steps(t0_ + 2 * pr), pro_steps(t0_ + 2 * pr + 1)
                                PS += [sa[0], sb_[0], sa[1], sb_[1], sa[2], sb_[2]]

                    def slot():
                        if PS:
                            PS.pop(0)()
                    ab = actb[m % 2]
                    for j in range(4):
                        slot()
                        bg = self.pb()
                        self.mm(ps[:, bg, :], [(Wg[i][:, k, j * 128:(j + 1) * 128], h2T[m][:, k, :]) for k in range(8)], [Wg[i], h2T[m]], [PB[bg]])
                        bu_ = self.pb()
                        self.mm(ps[:, bu_, :], [(Wu[i][:, k, j * 128:(j + 1) * 128], h2T[m][:, k, :]) for k in range(8)], [Wu[i], h2T[m]], [PB[bu_]])
                        s_ = sg[j % 2]
                        self.act(s_[:], ps[:, bg, :], AF.Silu, [PB[bg]], [s_])
                        self.tt("dve", ab[:, j, :], ps[:, bu_, :], s_[:], ALU.mult, [PB[bu_], s_], [ab])
                    for s4 in range(4):
                        t = m * 4 + s4
                        for h in range(2):
                            slot()
                            bk = self.pb()
                            self.mm(ps[:, bk, :], [(ab[:, j, s4 * 128:(s4 + 1) * 128], Wd[i][:, j, h * 512:(h + 1) * 512]) for j in range(4)], [ab, Wd[i]], [PB[bk]])
                            self.stt(xall[t][:, h * 512:(h + 1) * 512], ps[:, bk, :], Gall[:, t, e:e + 1], xall[t][:, h * 512:(h + 1) * 512],
                                     ALU.mult, ALU.add, [PB[bk], GR[t], xall[t]], [xall[t]])
                    if e == 31:
                        for t in range(4 * m, 4 * m + 4):
                            epilogue(t)
            tk.barrier()


def _col(v, nchunk):
    return np.ascontiguousarray(v.reshape(nchunk, 128).T)


def prep_shared(inp, nl=DEPTH):
    f = np.float32
    L = DEPTH
    sh = {}
    sh["w_ada"] = np.ascontiguousarray(inp["w_ada"], dtype=f)
    sh["badac"] = np.ascontiguousarray(np.stack([_col(inp["b_ada"][l], 48) for l in range(L)], axis=1), dtype=f)
    sh["n1c"] = np.ascontiguousarray(np.stack([_col(inp["norm1_w"][l], 8) for l in range(L)], axis=1), dtype=f)
    sh["n2c"] = np.ascontiguousarray(np.stack([_col(inp["norm2_w"][l], 8) for l in range(L)], axis=1), dtype=f)
    sh["w_in"] = np.ascontiguousarray(inp["w_in"], dtype=f)
    sh["w_out"] = np.ascontiguousarray(inp["w_out"], dtype=f)
    sh["w_glu"] = np.ascontiguousarray(inp["w_glu"], dtype=f)
    cw = np.zeros((128, L, 12, 4), f)
    cb = np.zeros((128, L, 12), f)
    yn = np.zeros((128, L, 12), f)
    for l in range(L):
        cw[:, l] = inp["conv_w"][l].T.reshape(12, 128, 4).transpose(1, 0, 2)
        cb[:, l] = _col(inp["conv_b"][l], 12)
        yn[:, l] = _col(np.concatenate([inp["ssd_norm_w"][l], inp["s5_norm_w"][l]]), 12)
    sh["convw"], sh["convb"], sh["ynormc"] = cw, cb, yn
    sh["rows1"] = np.ascontiguousarray(np.concatenate(
        [inp["dt_bias"], inp["a_log"], inp["d_ssd"], inp["b_glu"], inp["s5_d"], inp["b_rg"], inp["b_re"]], axis=1), dtype=f)
    sh["rows2"] = np.ascontiguousarray(np.concatenate(
        [inp["s5_log_dt"], inp["s5_lam_re"].reshape(L, -1), inp["s5_lam_im"].reshape(L, -1)], axis=1), dtype=f)
    sh["fnw"] = np.ascontiguousarray(inp["final_norm_w"].reshape(1, D), dtype=f)
    lamc = np.zeros((128, L, 3, 16), f)
    bblk = np.zeros((L, 2, 128, 16, 128), f)
    cblk = np.zeros((L, 2, 128, 16, 32), f)
    for l in range(L):
        for gp in range(16):
            for g2 in range(2):
                g = 2 * gp + g2
                lamc[g2 * 64:(g2 + 1) * 64, l, 0, gp] = inp["s5_lam_re"][l, g]
                lamc[g2 * 64:(g2 + 1) * 64, l, 1, gp] = inp["s5_lam_im"][l, g]
                lamc[g2 * 64:(g2 + 1) * 64, l, 2, gp] = inp["s5_log_dt"][l, g]
                gl = (gp % 4) * 2 + g2
                for ri, nm in enumerate(("s5_b_re", "s5_b_im")):
                    bblk[l, ri, gl * 16:(gl + 1) * 16, gp, g2 * 64:(g2 + 1) * 64] = inp[nm][l, g].T
                for ri, nm in enumerate(("s5_c_re", "s5_c_im")):
                    cblk[l, ri, g2 * 64:(g2 + 1) * 64, gp, g2 * 16:(g2 + 1) * 16] = inp[nm][l, g].T
    sh["lamc"], sh["bblk"], sh["cblk"] = lamc, bblk, cblk
    sh["wr"] = np.ascontiguousarray(np.concatenate([inp["w_rg"], inp["w_re"]], axis=2), dtype=f)
    sh["w_eg"] = np.ascontiguousarray(inp["w_eg"], dtype=f)
    sh["w_eu"] = np.ascontiguousarray(inp["w_eu"], dtype=f)
    sh["w_ed"] = np.ascontiguousarray(inp["w_ed"], dtype=f)
    cst = np.zeros((128, 4, 128), f)
    cst[:, 0] = np.eye(128, dtype=f)
    cst[:, 1] = np.triu(np.ones((128, 128), f))
    cst[:, 2] = np.where(np.triu(np.ones((128, 128))) > 0, 0.0, -30000.0)
    cst[:, 3] = np.arange(1, 129, dtype=f)[None, :]
    sh["cst"] = cst
    return sh


_NC_CACHE = {}


def run(inp, nl=DEPTH, trace=False):
    inp = {k: np.asarray(v) for k, v in inp.items()}
    if nl not in _NC_CACHE:
        _NC_CACHE[nl] = B(nl).build()
    nc = _NC_CACHE[nl]
    sh = prep_shared(inp)
    in_maps = []
    for b in range(8):
        m = dict(sh)
        m["x"] = np.ascontiguousarray(inp["x"][b], dtype=np.float32)
        m["ccol"] = _col(np.asarray(inp["c"][b], dtype=np.float32), 8)
        in_maps.append(m)
    res = run_bass_kernel_spmd(nc, in_maps, core_ids=list(range(8)))
    return np.stack([np.asarray(r["y"]) for r in res.results], axis=0).astype(np.float32)


def kernel(**inputs):
    return run(inputs, DEPTH)
```
